# Optimizing a Trainium2 kernel written in Bass

```python
import jax, jax.numpy as jnp
from jax import lax
import numpy as np

D_MODEL = 1024
BATCH = 4
SEQ = 8192
DEPTH = 1

CHUNK = 64
MIX_WIDTH = D_MODEL
LRU_WIDTH = MIX_WIDTH // 2
LRU_HEADS = 8
LRU_HEAD_DIM = LRU_WIDTH // LRU_HEADS
CONV_WIDTH = 4
LRU_C = 8.0
RWKV_WIDTH = MIX_WIDTH - LRU_WIDTH
RWKV_HEAD_DIM = 64
RWKV_HEADS = RWKV_WIDTH // RWKV_HEAD_DIM
DECAY_LORA = 64
AAA_LORA = 64
GATE_LORA = 128
RWKV_PROJ = 3 * RWKV_WIDTH + DECAY_LORA + AAA_LORA + GATE_LORA
PROJ_WIDTH = 2 * LRU_WIDTH + RWKV_PROJ
N_GROUPS = 4
EXPERTS_PER_GROUP = 8
N_EXPERTS = N_GROUPS * EXPERTS_PER_GROUP
TOP_K = 2
D_EXPERT = 512
RMS_EPS = 1e-6
GN_EPS = 64e-5

kernel_name = 'hybrid_rglru_rwkv7_hmoe_adaln'


def _rmsnorm(x, g):
    x32 = x.astype(jnp.float32)
    y = x32 * lax.rsqrt(jnp.mean(x32 * x32, axis=-1, keepdims=True) + RMS_EPS)
    return (y * g.astype(jnp.float32)).astype(x.dtype)


def _adaln(x, g, shift, scale):
    return (_rmsnorm(x, g) * (1 + scale[:, None, :]) + shift[:, None, :]).astype(x.dtype)


def _lin_combine(e1, e2):
    a1, b1 = e1
    a2, b2 = e2
    return a1 * a2, a2 * b1 + b2


def _rglru_group(u_x, u_gate, conv_w, conv_b, wa, ba, wi, bi, lam, norm_g):
    f32 = jnp.float32
    bsz, t, w = u_x.shape
    xc = lax.conv_general_dilated(u_x, conv_w[:, None, :].astype(u_x.dtype), (1,), [(CONV_WIDTH - 1, 0)],
                                  dimension_numbers=('NWC', 'WIO', 'NWC'), feature_group_count=w)
    xc = (xc + conv_b).astype(f32)
    xh = xc.reshape(bsz, t, LRU_HEADS, LRU_HEAD_DIM)
    r = jax.nn.sigmoid(jnp.einsum('bthi,hij->bthj', xh, wa.astype(f32)) + ba.astype(f32)).reshape(bsz, t, w)
    i = jax.nn.sigmoid(jnp.einsum('bthi,hij->bthj', xh, wi.astype(f32)) + bi.astype(f32)).reshape(bsz, t, w)
    log_a = -LRU_C * r * jax.nn.softplus(-lam.astype(f32))
    a = jnp.exp(log_a)
    b = jnp.sqrt(-jnp.expm1(2.0 * log_a)) * (i * xc)
    _, h = lax.associative_scan(_lin_combine, (a, b), axis=1)
    y = h * jax.nn.gelu(u_gate.astype(f32))
    yh = y.reshape(bsz, t, LRU_HEADS, LRU_HEAD_DIM)
    yh = yh * lax.rsqrt(jnp.mean(yh * yh, axis=-1, keepdims=True) + RMS_EPS)
    return yh.reshape(bsz, t, w) * norm_g.astype(f32)


def _rwkv7_group(u, mu, w0, w_up, a0, a_up, g_up, k_k, k_a, r_k, ln_w, ln_b):
    f32 = jnp.float32
    bsz, t, _ = u.shape
    u = u.astype(f32)
    prev = jnp.pad(u, ((0, 0), (1, 0), (0, 0)))[:, :-1]
    u = u + (prev - u) * mu.astype(f32)
    s = [RWKV_WIDTH, 2 * RWKV_WIDTH, 3 * RWKV_WIDTH, 3 * RWKV_WIDTH + DECAY_LORA,
         3 * RWKV_WIDTH + DECAY_LORA + AAA_LORA]
    r, k, v, wd, ad, gd = jnp.split(u, s, axis=-1)
    w = -jax.nn.softplus(-(w0.astype(f32) + jnp.tanh(wd) @ w_up.astype(f32))) - 0.5
    decay = jnp.exp(-jnp.exp(w))
    a = jax.nn.sigmoid(a0.astype(f32) + ad @ a_up.astype(f32))
    g = jax.nn.sigmoid(gd) @ g_up.astype(f32)

    def heads(z):
        return z.reshape(bsz, t, RWKV_HEADS, RWKV_HEAD_DIM)

    kk = heads(k * k_k.astype(f32))
    kk = kk / jnp.maximum(jnp.sqrt(jnp.sum(kk * kk, axis=-1, keepdims=True)), 1e-12)
    k = k * (1 + (a - 1) * k_a.astype(f32))
    r, k, v, decay, a = heads(r), heads(k), heads(v), heads(decay), heads(a)
    n_chunks = t // CHUNK

    def to_chunks(z):
        return z.transpose(1, 0, 2, 3).reshape(n_chunks, CHUNK, bsz, RWKV_HEADS, RWKV_HEAD_DIM)

    def step(state, inp):
        r_t, w_t, k_t, v_t, kk_t, a_t = inp
        sa = jnp.einsum('bhvk,bhk->bhv', state, -kk_t)
        state = (state * w_t[:, :, None, :] + sa[..., None] * (kk_t * a_t)[:, :, None, :]
                 + v_t[..., None] * k_t[:, :, None, :])
        return state, jnp.einsum('bhvk,bhk->bhv', state, r_t)

    def chunk_step(state, chunk_inp):
        return lax.scan(step, state, chunk_inp)

    s0 = jnp.zeros((bsz, RWKV_HEADS, RWKV_HEAD_DIM, RWKV_HEAD_DIM), f32)
    xs = (to_chunks(r), to_chunks(decay), to_chunks(k), to_chunks(v), to_chunks(kk), to_chunks(a))
    _, o = lax.scan(chunk_step, s0, xs)
    o = o.reshape(t, bsz, RWKV_HEADS, RWKV_HEAD_DIM).transpose(1, 0, 2, 3)
    mean = jnp.mean(o, axis=-1, keepdims=True)
    var = jnp.mean(jnp.square(o - mean), axis=-1, keepdims=True)
    hshape = (RWKV_HEADS, RWKV_HEAD_DIM)
    o = (o - mean) * lax.rsqrt(var + GN_EPS) * ln_w.astype(f32).reshape(hshape) + ln_b.astype(f32).reshape(hshape)
    bonus = jnp.sum(r * k * r_k.astype(f32), axis=-1, keepdims=True) * v
    return (o + bonus).reshape(bsz, t, RWKV_WIDTH) * g


def _hier_moe(h, w_grp, b_grp, w_exp, b_exp, w1, w3, w2):
    f32 = jnp.float32
    bsz, t, d = h.shape
    hf = h.reshape(-1, d)
    n = hf.shape[0]
    grp_p = jax.nn.softmax((hf @ w_grp).astype(f32) + b_grp.astype(f32), axis=-1)
    g_w, g_idx = lax.top_k(grp_p, 1)
    exp_logits = ((hf @ w_exp).astype(f32) + b_exp.astype(f32)).reshape(n, N_GROUPS, EXPERTS_PER_GROUP)
    in_grp = jnp.take_along_axis(exp_logits, g_idx[:, :, None], axis=1)[:, 0]
    top_v, top_i = lax.top_k(in_grp, TOP_K)
    e_w = jax.nn.softmax(top_v, axis=-1) * g_w
    e_id = g_idx * EXPERTS_PER_GROUP + top_i
    flat_e = e_id.reshape(-1)
    flat_tok = jnp.repeat(jnp.arange(n, dtype=jnp.int32), TOP_K)
    order = jnp.argsort(flat_e)
    tok = flat_tok[order]
    sizes = jnp.bincount(flat_e, length=N_EXPERTS).astype(jnp.int32)
    xs = hf[tok]
    hid = jax.nn.silu(lax.ragged_dot(xs, w1, sizes)) * lax.ragged_dot(xs, w3, sizes)
    ys = lax.ragged_dot(hid, w2, sizes)
    ys = ys * e_w.reshape(-1)[order][:, None].astype(ys.dtype)
    out = jnp.zeros_like(hf).at[tok].add(ys)
    return out.reshape(bsz, t, d)


def setup_inputs(seed: int = 0) -> dict:
    key = jax.random.key(seed)
    ks = iter(jax.random.split(key, 48))
    f32 = jnp.float32
    L = DEPTH

    def nrm(shape, scale):
        return jax.random.normal(next(ks), shape, f32) * scale

    x = nrm((BATCH, SEQ, D_MODEL), 1.0)
    c = nrm((BATCH, D_MODEL), 1.0)
    w_ada = nrm((L, D_MODEL, 6 * D_MODEL), 0.3 * D_MODEL ** -0.5)
    b_ada = nrm((L, 6 * D_MODEL), 0.02)
    norm1_g = 1.0 + nrm((L, D_MODEL), 0.02)
    w_in = nrm((L, D_MODEL, PROJ_WIDTH), D_MODEL ** -0.5)
    conv_w = nrm((L, CONV_WIDTH, LRU_WIDTH), CONV_WIDTH ** -0.5)
    conv_b = nrm((L, LRU_WIDTH), 0.02)
    lru_wa = nrm((L, LRU_HEADS, LRU_HEAD_DIM, LRU_HEAD_DIM), LRU_HEAD_DIM ** -0.5)
    lru_ba = nrm((L, LRU_HEADS, LRU_HEAD_DIM), 0.02)
    lru_wi = nrm((L, LRU_HEADS, LRU_HEAD_DIM, LRU_HEAD_DIM), LRU_HEAD_DIM ** -0.5)
    lru_bi = nrm((L, LRU_HEADS, LRU_HEAD_DIM), 0.02)
    u = jax.random.uniform(next(ks), (L, LRU_WIDTH), f32, 0.9, 0.999)
    a_base = u ** (1.0 / LRU_C)
    lru_lam = jnp.log(a_base) - jnp.log1p(-a_base)
    lru_norm_g = 1.0 + nrm((L, LRU_WIDTH), 0.02)
    tok_mu = jax.random.uniform(next(ks), (L, RWKV_PROJ), f32, 0.0, 1.0)
    w0 = jnp.linspace(-6.5, -1.5, RWKV_WIDTH, dtype=f32)[None, :] + nrm((L, RWKV_WIDTH), 0.1)
    w_up = nrm((L, DECAY_LORA, RWKV_WIDTH), 0.5 * DECAY_LORA ** -0.5)
    a0 = nrm((L, RWKV_WIDTH), 0.1)
    a_up = nrm((L, AAA_LORA, RWKV_WIDTH), AAA_LORA ** -0.5)
    g_up = nrm((L, GATE_LORA, RWKV_WIDTH), GATE_LORA ** -0.5)
    k_k = 0.85 + nrm((L, RWKV_WIDTH), 0.02)
    k_a = 1.0 + nrm((L, RWKV_WIDTH), 0.02)
    r_k = -0.04 + nrm((L, RWKV_HEADS, RWKV_HEAD_DIM), 0.1)
    ln_x_w = 1.0 + nrm((L, RWKV_WIDTH), 0.02)
    ln_x_b = nrm((L, RWKV_WIDTH), 0.02)
    w_out = nrm((L, MIX_WIDTH, D_MODEL), MIX_WIDTH ** -0.5)
    norm2_g = 1.0 + nrm((L, D_MODEL), 0.02)
    w_grp = nrm((L, D_MODEL, N_GROUPS), D_MODEL ** -0.5)
    b_grp = nrm((L, N_GROUPS), 0.01)
    w_exp = nrm((L, D_MODEL, N_EXPERTS), D_MODEL ** -0.5)
    b_exp = nrm((L, N_EXPERTS), 0.01)
    w1 = nrm((L, N_EXPERTS, D_MODEL, D_EXPERT), D_MODEL ** -0.5)
    w3 = nrm((L, N_EXPERTS, D_MODEL, D_EXPERT), D_MODEL ** -0.5)
    w2 = nrm((L, N_EXPERTS, D_EXPERT, D_MODEL), D_EXPERT ** -0.5)
    final_g = 1.0 + nrm((D_MODEL,), 0.02)
    return {'x': x, 'c': c, 'w_ada': w_ada, 'b_ada': b_ada, 'norm1_g': norm1_g, 'w_in': w_in,
            'conv_w': conv_w, 'conv_b': conv_b, 'lru_wa': lru_wa, 'lru_ba': lru_ba, 'lru_wi': lru_wi,
            'lru_bi': lru_bi, 'lru_lam': lru_lam, 'lru_norm_g': lru_norm_g, 'tok_mu': tok_mu, 'w0': w0,
            'w_up': w_up, 'a0': a0, 'a_up': a_up, 'g_up': g_up, 'k_k': k_k, 'k_a': k_a, 'r_k': r_k,
            'ln_x_w': ln_x_w, 'ln_x_b': ln_x_b, 'w_out': w_out, 'norm2_g': norm2_g, 'w_grp': w_grp,
            'b_grp': b_grp, 'w_exp': w_exp, 'b_exp': b_exp, 'w1': w1, 'w3': w3, 'w2': w2,
            'final_g': final_g}


def reference(x, c, w_ada, b_ada, norm1_g, w_in, conv_w, conv_b, lru_wa, lru_ba, lru_wi, lru_bi,
              lru_lam, lru_norm_g, tok_mu, w0, w_up, a0, a_up, g_up, k_k, k_a, r_k, ln_x_w, ln_x_b,
              w_out, norm2_g, w_grp, b_grp, w_exp, b_exp, w1, w3, w2, final_g):
    cond = jax.nn.silu(c)
    for l in range(DEPTH):
        mod = cond @ w_ada[l] + b_ada[l]
        sh1, sc1, g1, sh2, sc2, g2 = jnp.split(mod, 6, axis=-1)
        h = _adaln(x, norm1_g[l], sh1, sc1)
        p = h @ w_in[l]
        y_lru = _rglru_group(p[..., :LRU_WIDTH], p[..., LRU_WIDTH:2 * LRU_WIDTH], conv_w[l], conv_b[l],
                             lru_wa[l], lru_ba[l], lru_wi[l], lru_bi[l], lru_lam[l], lru_norm_g[l])
        y_rwkv = _rwkv7_group(p[..., 2 * LRU_WIDTH:], tok_mu[l], w0[l], w_up[l], a0[l], a_up[l], g_up[l],
                              k_k[l], k_a[l], r_k[l], ln_x_w[l], ln_x_b[l])
        y = jnp.concatenate([y_lru, y_rwkv], axis=-1).astype(x.dtype) @ w_out[l]
        x = x + g1[:, None, :] * y
        h2 = _adaln(x, norm2_g[l], sh2, sc2)
        x = x + g2[:, None, :] * _hier_moe(h2, w_grp[l], b_grp[l], w_exp[l], b_exp[l], w1[l], w3[l], w2[l])
    return _rmsnorm(x, final_g)
```

```python
import numpy as np
from contextlib import ExitStack
import concourse.bass as bass
import concourse.mybir as mybir
from concourse.bass_utils import run_bass_kernel_spmd

F32 = mybir.dt.float32
BF16 = mybir.dt.bfloat16
U32 = mybir.dt.uint32
AF = mybir.ActivationFunctionType
ALU = mybir.AluOpType
AX = mybir.AxisListType

D = 1024
SEQ = 8192
NB = 4
PROJ = 2816
NCH = 22
CH = 128
NE = 32
DE = 512
RMS_EPS = 1e-6
GN_EPS = 64e-5


class Tl:
    def __init__(self, t, name=""):
        self.t = t
        self.name = name
        self.w = None
        self.r = []
        self.wstream = None
        self.psum = name.startswith("ps")

    def __getitem__(self, idx):
        return V(self, self.t[idx])


class V:
    __slots__ = ("tl", "ap")

    def __init__(self, tl, ap):
        self.tl = tl
        self.ap = ap


class Prog:
    STREAMS = ("pe", "act", "dve", "pool", "sp")

    def __init__(self, nc):
        self.nc = nc
        self.ops = {s: [] for s in self.STREAMS}
        self.count = {}
        self.seen = {s: {} for s in self.STREAMS}
        self.semkeys = []

    def _tok(self, key, amt):
        self.count[key] = self.count.get(key, 0) + amt
        if key not in self.semkeys:
            self.semkeys.append(key)
        return (key, self.count[key])

    def op(self, stream, fn, reads=(), writes=(), dma=None):
        waits = {}

        def need(tok, tstream):
            if tok is None:
                return
            key, val = tok
            if tstream == stream and stream == "pe":
                return
            if self.seen[stream].get(key, 0) >= val:
                return
            if waits.get(key, 0) < val:
                waits[key] = val

        for tl in reads:
            need(tl.w, tl.wstream)
            if tl.psum:
                for (k, v, s) in tl.r:
                    if s != stream:
                        need((k, v), s)
        for tl in writes:
            need(tl.w, tl.wstream)
            for (k, v, s) in tl.r:
                if s == stream and dma is None:
                    continue
                need((k, v), s)
        for k, v in waits.items():
            self.seen[stream][k] = v
        if dma is not None:
            tok = self._tok("q_" + dma, 16)
            tstream = "dma"
        else:
            tok = self._tok("s_" + stream, 1)
            tstream = stream
        self.ops[stream].append((list(waits.items()), fn, tok[0], 16 if dma is not None else 1))
        for tl in reads:
            tl.r.append((tok[0], tok[1], tstream))
        for tl in writes:
            tl.w = tok
            tl.wstream = tstream
            tl.r = []
        return tok

    def wait_all(self, stream, toks):
        waits = {}
        for key, val in toks:
            if self.seen[stream].get(key, 0) < val and waits.get(key, 0) < val:
                waits[key] = val
        for k, v in waits.items():
            self.seen[stream][k] = v
        self.ops[stream].append((list(waits.items()), None, None, 0))

    def emit(self):
        nc = self.nc
        with ExitStack() as es:
            sems = {k: es.enter_context(nc.semaphore(k)) for k in self.semkeys}
            block = es.enter_context(nc.Block())

            def replay(stream, eng):
                for waits, fn, key, amt in self.ops[stream]:
                    for k, v in waits:
                        eng.wait_ge(sems[k], v)
                    if fn is not None:
                        fn(eng).then_inc(sems[key], amt)

            @block.tensor
            def _(e):
                replay("pe", e)

            @block.scalar
            def _(e):
                replay("act", e)

            @block.vector
            def _(e):
                replay("dve", e)

            @block.gpsimd
            def _(e):
                replay("pool", e)

            @block.sync
            def _(e):
                replay("sp", e)

    def barrier(self):
        toks = [(k, self.count[k]) for k in self.semkeys]
        for s in self.STREAMS:
            self.wait_all(s, toks)


def _tls(*vs):
    return [v.tl for v in vs if isinstance(v, V)]


def _ap(v):
    return v.ap if isinstance(v, V) else v


_PP_ITEMS = [("n1g", 8), ("n2g", 8), ("bsh1", 8), ("bsc1", 8), ("bsh2", 8), ("bsc2", 8), ("cw", 16),
             ("cb", 4), ("ba", 4), ("bi", 4), ("lam", 4), ("lg", 4), ("mu", 14), ("w0", 4), ("a0", 4),
             ("kk", 4), ("ka", 4), ("rk", 4), ("lnw", 4), ("lnb", 4)]
PP = {}
_o = 0
for _n, _c in _PP_ITEMS:
    PP[_n] = _o
    _o += _c
NPP = _o
C_ID, C_BONES, C_TRIS, C_TRII, C_ONES, C_TRIL4 = 0, 128, 256, 384, 512, 640
NCONST = 1152
HALO = 4
LDK = 0.6065306597126334


class KB:
    def __init__(self, T, debug=None, stop=99, ne=NE, mstop=99):
        self.T = T
        self.ne = ne
        self.mstop = mstop
        self.stop = stop
        self.NBLK = T // 128
        self.NOWN = T // 2
        self.debug = debug
        self.nc = bass.Bass("TRN2", target_bir_lowering=False)
        self.P = Prog(self.nc)
        self._nf = 0
        self._nb = 0
        self._dram_names = set()

    def sb(self, es, name, shape, dt):
        self._uid = getattr(self, "_uid", 0) + 1
        return Tl(es.enter_context(self.nc.sbuf_tensor("%s_u%d" % (name, self._uid), shape, dt)), name)

    def dram(self, name, shape, dt, kind):
        self._dram_names.add(name)
        return Tl(self.nc.dram_tensor(name, shape, dt, kind=kind).ap(), name)

    def psf(self):
        t = self.psF[self._nf % len(self.psF)]
        self._nf += 1
        return t

    def psb(self):
        t = self.psB[self._nb % len(self.psB)]
        self._nb += 1
        return t

    def ACT(self, out, in_, func, scale=None, bias=None, accum=None):
        kw = {}
        if scale is not None:
            kw["scale"] = _ap(scale)
        if bias is not None:
            kw["bias"] = _ap(bias)
        if accum is not None:
            kw["accum_out"] = accum.ap
        o, i = out.ap, in_.ap
        return self.P.op("act", lambda e: e.activation(out=o, in_=i, func=func, **kw),
                         _tls(in_, scale, bias), _tls(out, accum))

    def TS(self, eng, out, in0, s1, op0, s2=None, op1=None):
        o, i, a1, a2 = out.ap, in0.ap, _ap(s1), _ap(s2)
        kw = {} if op1 is None else {"op1": op1}
        return self.P.op(eng, lambda e: e.tensor_scalar(out=o, in0=i, scalar1=a1, scalar2=a2, op0=op0, **kw),
                         _tls(in0, s1, s2), _tls(out))

    def STT(self, out, in0, sc, in1, op0, op1):
        o, i0, s, i1 = out.ap, in0.ap, _ap(sc), in1.ap
        return self.P.op("dve", lambda e: e.scalar_tensor_tensor(out=o, in0=i0, scalar=s, in1=i1, op0=op0, op1=op1),
                         _tls(in0, sc, in1), _tls(out))

    def TT(self, eng, out, in0, in1, op):
        o, i0, i1 = out.ap, in0.ap, in1.ap
        return self.P.op(eng, lambda e: e.tensor_tensor(out=o, in0=i0, in1=i1, op=op), _tls(in0, in1), _tls(out))

    def CP(self, eng, out, in_):
        o, i = out.ap, in_.ap
        if eng == "act":
            return self.P.op("act", lambda e: e.copy(out=o, in_=i), _tls(in_), _tls(out))
        return self.P.op(eng, lambda e: e.tensor_copy(out=o, in_=i), _tls(in_), _tls(out))

    def MSET(self, eng, out, val):
        o = out.ap
        return self.P.op(eng, lambda e: e.memset(o, val), [], _tls(out))

    def RECIP(self, out, in_):
        o, i = out.ap, in_.ap
        return self.P.op("dve", lambda e: e.reciprocal(out=o, in_=i), _tls(in_), _tls(out))

    def MM(self, out, lhsT, rhs, start=True, stop=True):
        o, l, r = out.ap, lhsT.ap, rhs.ap
        return self.P.op("pe", lambda e: e.matmul(o, lhsT=l, rhs=r, start=start, stop=stop),
                         _tls(lhsT, rhs), _tls(out))

    def TR(self, out, in_, ident):
        o, i, d = out.ap, in_.ap, ident.ap
        return self.P.op("pe", lambda e: e.transpose(out=o, in_=i, identity=d), _tls(in_, ident), _tls(out))

    def DMA(self, stream, q, out, in_):
        o, i = out.ap, in_.ap
        side = in_.tl if out.tl.name in self._dram_names else out.tl
        return self.P.op(stream, lambda e: e.dma_start(out=o, in_=i), _tls(in_), _tls(out), dma=side.name)

    def RED(self, out, in_, op):
        o, i = out.ap, in_.ap
        return self.P.op("dve", lambda e: e.tensor_reduce(out=o, in_=i, axis=AX.X, op=op), _tls(in_), _tls(out))

    def MAX8(self, out, in_):
        o, i = out.ap, in_.ap
        return self.P.op("dve", lambda e: e.max(out=o, in_=i), _tls(in_), _tls(out))

    def SCAN(self, out, d0, d1, init, op0, op1):
        o, a, b, c = out.ap, d0.ap, d1.ap, _ap(init)
        return self.P.op("dve", lambda e: e.tensor_tensor_scan(out=o, data0=a, data1=b, initial=c, op0=op0, op1=op1),
                         _tls(d0, d1, init), _tls(out))

    def build(self):
        nc, P, T, NBLK, NOWN = self.nc, self.P, self.T, self.NBLK, self.NOWN
        ACT, TS, STT, TT, CP, MM, TR, DMA, SCAN, RECIP, MSET = (self.ACT, self.TS, self.STT, self.TT, self.CP,
                                                                self.MM, self.TR, self.DMA, self.SCAN, self.RECIP,
                                                                self.MSET)
        mult, add, sub = ALU.mult, ALU.add, ALU.subtract
        xs = self.dram("xs", [T, D], F32, "ExternalInput")
        cTd = self.dram("cT", [128, 8], F32, "ExternalInput")
        w_ada = self.dram("w_ada", [D, 6 * D], F32, "ExternalInput")
        ppd = self.dram("pp", [128, NPP], F32, "ExternalInput")
        w_in = self.dram("w_in", [D, PROJ], F32, "ExternalInput")
        w_out = self.dram("w_out", [D, D], F32, "ExternalInput")
        bdd = self.dram("bd", [128, 8, 128], F32, "ExternalInput")
        lupd = self.dram("lup", [128, 3, 512], F32, "ExternalInput")
        bcAd = self.dram("bcA", [128, 2048], F32, "ExternalInput")
        bcBd = self.dram("bcB", [128, 1024 + 36], F32, "ExternalInput")
        wrd = self.dram("wr", [128, 8, 36], F32, "ExternalInput")
        cstd = self.dram("consts", [128, NCONST], F32, "ExternalInput")
        bmd = self.dram("blkmask", [128, NBLK], F32, "ExternalInput")
        w1d = w3d = w2d = None
        if self.debug != "nomoe":
            w1d = self.dram("w1", [self.ne, D, DE], F32, "ExternalInput")
            w3d = self.dram("w3", [self.ne, D, DE], F32, "ExternalInput")
            w2d = self.dram("w2", [self.ne, DE, D], F32, "ExternalInput")
        xnew = self.dram("xnew", [NOWN, D], F32, "Internal")
        outd = self.dram("out", [NOWN, D], F32, "ExternalOutput")

        es_all = ExitStack()
        es = ExitStack()
        self.psF = [Tl(es_all.enter_context(nc.psum_tensor("psf%d" % i, [128, 512], F32)), "psf%d" % i) for i in range(6)]
        self.psB = [Tl(es_all.enter_context(nc.psum_tensor("psb%d" % i, [128, 1024], BF16)), "psb%d" % i) for i in range(2)]
        psf, psb = self.psf, self.psb
        sb = self.sb

        cst = sb(es, "cst", [128, NCONST], F32)
        pp = sb(es, "ppt", [128, NPP], F32)
        bd = sb(es, "bdt", [128, 8, 128], F32)
        lup = sb(es, "lupt", [128, 3, 512], F32)
        bcB = sb(es, "bcBt", [128, 1024 + 36], F32)
        wr = sb(es, "wrt", [128, 8, 36], F32)
        bm = sb(es, "bmt", [128, NBLK], F32)
        identb = sb(es, "identb", [128, 128], BF16)
        condT = sb(es, "condT", [128, 8], F32)
        modT = sb(es, "modT", [128, 32], F32)
        s1T = sb(es, "s1T", [128, 8], F32)
        s2T = sb(es, "s2T", [128, 8], F32)
        g1b = sb(es, "g1b", [128, D], F32)
        g2b = sb(es, "g2b", [128, D], F32)
        cbm = sb(es, "cbm", [128, 4, NBLK], F32)
        cA = sb(es, "cA", [128, 4], F32)
        omka = sb(es, "omka", [128, 4], F32)

        def ppc(name, i=0):
            return pp[:, PP[name] + i:PP[name] + i + 1]

        ident = cst[:, C_ID:C_ID + 128]
        bones = cst[:, C_BONES:C_BONES + 128]
        maskSI = cst[:, C_TRIS:C_TRIS + 256]
        ones = cst[:, C_ONES:C_ONES + 128]
        maskL4 = cst[:, C_TRIL4:C_TRIL4 + 512]

        DMA("sp", "c", cst[:, :], cstd[:, :])
        DMA("sp", "c", pp[:, :], ppd[:, :])
        DMA("sp", "c", condT[:, :], cTd[:, :])
        DMA("sp", "c", bd[:, :, :], bdd[:, :, :])
        DMA("sp", "c", lup[:, :, :], lupd[:, :, :])
        DMA("sp", "c", bcB[:, :], bcBd[:, :])
        DMA("sp", "c", wr[:, :, :], wrd[:, :, :])
        DMA("sp", "c", bm[:, :], bmd[:, :])
        es_m = ExitStack()
        w_in_sb = sb(es_m, "w_in_sb", [128, 8, PROJ], BF16)
        w_out_sb = sb(es_m, "w_out_sb", [128, 8, D], BF16)
        es_s = ExitStack()
        wst = [sb(es_s, "wst%d" % i, [128, 8, 512], F32) for i in range(2)]
        condbc = sb(es_s, "condbc", [128, 8, 128], F32)
        bcA = sb(es_s, "bcAt", [128, 2048], F32)
        DMA("sp", "c", bcA[:, :], bcAd[:, :])
        for kc in range(8):
            for hh in range(2):
                c0 = hh * 1408
                DMA("pool", "w", w_in_sb[:, kc, c0:c0 + 1408], w_in[kc * 128:(kc + 1) * 128, c0:c0 + 1408])
        for kc in range(8):
            DMA("pool", "w", w_out_sb[:, kc, :], w_out[kc * 128:(kc + 1) * 128, :])
        CP("dve", identb[:, :], ident)
        ACT(condT[:, :], condT[:, :], AF.Silu)
        for kc in range(8):
            TS("dve", condbc[:, kc, :], ones, condT[:, kc:kc + 1], mult)
        psMod = psf()
        fm = {0: 0, 1: 0, 2: 1, 3: 1, 6: 2, 7: 2, 8: 3, 9: 3}
        for gi in range(12):
            wb = wst[gi % 2]
            src = V(w_ada, w_ada.t[:, gi * 512:(gi + 1) * 512].rearrange("(k p) n -> p k n", p=128))
            DMA("sp", "c", wb[:, :, :], src)
            if gi in fm:
                for n4 in range(4):
                    col = fm[gi] * 8 + (gi % 2) * 4 + n4
                    for kc in range(8):
                        MM(psMod[:, col:col + 1], wb[:, kc, n4 * 128:(n4 + 1) * 128], condT[:, kc:kc + 1],
                           start=(kc == 0), stop=(kc == 7))
            else:
                ps = psf()
                for kc in range(8):
                    MM(ps[:, 0:512], condbc[:, kc, :], wb[:, kc, :], start=(kc == 0), stop=(kc == 7))
                dst = g1b if gi < 6 else g2b
                c0 = (gi % 2) * 512
                b0 = (0 if gi < 6 else 1024) + c0
                TT("dve", dst[:, c0:c0 + 512], ps[:, 0:512], bcA[:, b0:b0 + 512], add)
        TT("dve", modT[:, :], psMod[:, 0:32], pp[:, PP["bsh1"]:PP["bsh1"] + 32], add)
        STT(s1T[:, :], modT[:, 8:16], 1.0, pp[:, PP["n1g"]:PP["n1g"] + 8], add, mult)
        STT(s2T[:, :], modT[:, 24:32], 1.0, pp[:, PP["n2g"]:PP["n2g"] + 8], add, mult)
        for j in range(4):
            TS("dve", cbm[:, j, :], bm[:, :], ppc("cb", j), mult)
        ACT(cA[:, :], pp[:, PP["lam"]:PP["lam"] + 4], AF.Exp, scale=-1.0)
        ACT(cA[:, :], cA[:, :], AF.Ln, bias=1.0)
        TS("dve", cA[:, :], cA[:, :], -8.0, mult)
        TS("dve", omka[:, :], pp[:, PP["ka"]:PP["ka"] + 4], -1.0, mult, 1.0, add)
        P.barrier()
        es_s.close()

        xt = [sb(es_m, "xt%d" % i, [128, D], F32) for i in range(2)]
        ss = sb(es_m, "ss", [128, 2], F32)
        rs = sb(es_m, "rs", [128, 2], F32)
        xn = [sb(es_m, "xn%d" % i, [128, D], BF16) for i in range(2)]
        hT = [sb(es_m, "hT%d" % i, [128, 8, 128], BF16) for i in range(2)]
        pU = [sb(es_m, "pU%d" % i, [128, 128 + HALO], F32) for i in range(4)]
        pG = [sb(es_m, "pG%d" % i, [128, 128], F32) for i in range(4)]
        pR = [sb(es_m, "pR%d" % i, [128, 128 + HALO], F32) for i in range(14)]
        us = [sb(es_m, "us%d" % i, [128, 128], F32) for i in range(14)]
        hprev = sb(es_m, "hprev", [128, 4], F32)
        WC = sb(es_m, "WC", [128, 4], F32)
        nbt = sb(es_m, "nbt", [128, 4], F32)
        ARz = [sb(es_m, "ARz%d" % i, [128, 4, 2, 128], BF16) for i in range(2)]
        BK = sb(es_m, "BK", [128, 4, 2, 128], BF16)
        BKp = sb(es_m, "BKp", [128, 4, 2, 128], BF16)
        vTb = sb(es_m, "vTb", [128, 4, 128], BF16)
        Vt = sb(es_m, "Vt", [128, 512], BF16)
        Bt = sb(es_m, "Bt", [128, 512], BF16)
        Kt = sb(es_m, "Kt", [128, 512], BF16)
        bon = sb(es_m, "bon", [128, 4, 128], F32)
        gT = sb(es_m, "gT", [128, 4, 128], F32)
        M1 = [sb(es_m, "M1_%d" % g, [128, 4, 512], BF16) for g in range(2)]
        NTb = [[sb(es_m, "NTb%d_%d" % (g, i), [128, 512], BF16) for i in range(2)] for g in range(2)]
        Nb = [[sb(es_m, "Nb%d_%d" % (g, i), [128, 512], BF16) for i in range(2)] for g in range(2)]
        Zb = [[sb(es_m, "Zb%d_%d" % (g, i), [128, 512], BF16) for i in range(2)] for g in range(2)]
        Xb = sb(es_m, "Xb", [128, 2, 256], BF16)
        Ub = sb(es_m, "Ub", [128, 2, 256], BF16)
        STf = sb(es_m, "STf", [128, 4, 64], F32)
        STb = [sb(es_m, "STb%d" % i, [128, 4, 64], BF16) for i in range(2)]
        OT = sb(es_m, "OT", [128, 512], F32)
        yT = [sb(es_m, "yT%d" % i, [128, 8, 128], BF16) for i in range(2)]
        xr = [sb(es_m, "xr0", [128, D], F32)] * 2
        xo = [sb(es_m, "xo0", [128, D], F32)] * 2
        tmps = {}

        def tmpj(slot, j):
            if (slot, j) not in tmps:
                tmps[(slot, j)] = sb(es_m, "t_%d_%d" % (slot, j), [128, 128], F32)
            return tmps[(slot, j)]

        def lockstep(gens):
            gens = list(gens)
            while gens:
                alive = []
                for g_ in gens:
                    try:
                        next(g_)
                        alive.append(g_)
                    except StopIteration:
                        pass
                gens = alive

        for t_ in pU + pR:
            MSET("pool", t_[:, :], 0.0)
        for t_ in ARz:
            MSET("pool", t_[:, :, :, :], 0.0)
        MSET("dve", hprev[:, :], 0.0)
        MSET("dve", STf[:, :, :], 0.0)
        MSET("dve", STb[0][:, :, :], 0.0)

        chunks = [(pU[i], HALO) for i in range(4)] + [(pG[i], 0) for i in range(4)] + [(pR[i], HALO) for i in range(14)]
        stop = self.stop
        for blk in range(NBLK if stop > 0 else 0):
            par = blk % 2
            r0 = blk * 128
            bmc = bm[:, blk:blk + 1]
            DMA("sp", "x", xt[par][:, :], xs[r0:r0 + 128, :])
            ACT(xn[par][:, :], xt[par][:, :], AF.Square, accum=ss[:, par:par + 1])
            ACT(rs[:, par:par + 1], ss[:, par:par + 1], AF.Sqrt, scale=1.0 / D, bias=RMS_EPS)
            RECIP(rs[:, par:par + 1], rs[:, par:par + 1])
            TS("dve", xn[par][:, :], xt[par][:, :], rs[:, par:par + 1], mult)
            pb = psb()
            for kc in range(8):
                TR(pb[:, kc * 128:(kc + 1) * 128], xn[par][:, kc * 128:(kc + 1) * 128], identb[:, :])
            for kc in range(8):
                ACT(hT[par][:, kc, :], pb[:, kc * 128:(kc + 1) * 128], AF.Identity,
                    scale=s1T[:, kc:kc + 1], bias=modT[:, kc:kc + 1])
            if stop <= 1:
                continue
            for ch, (tile, hal) in enumerate(chunks):
                if hal:
                    CP("pool", tile[:, 0:HALO], tile[:, 128:128 + HALO])
                ps = psf()
                for kc in range(8):
                    MM(ps[:, 0:128], w_in_sb[:, kc, ch * 128:(ch + 1) * 128], hT[par][:, kc, :],
                       start=(kc == 0), stop=(kc == 7))
                if ch % 2 == 0:
                    TS("dve", tile[:, hal:hal + 128], ps[:, 0:128], bmc, mult)
                else:
                    ACT(tile[:, hal:hal + 128], ps[:, 0:128], AF.Identity, scale=bmc)
            yTp = yT[par]
            if stop <= 2:
                continue
            def lru_chain(j):
                ux = pU[j]
                xc, rg, ig, aa, a2, bx, hs, gl, yy = (tmpj(s_, j) for s_ in range(9))
                y2, rn = rg, ig
                TS("dve", xc[:, :], ux[:, 1:129], ppc("cw", j), mult, cbm[:, j, blk:blk + 1], add)
                yield
                for tap in range(1, 4):
                    STT(xc[:, :], ux[:, 1 + tap:129 + tap], ppc("cw", tap * 4 + j), xc[:, :], mult, add)
                    yield
                psA = psf()
                MM(psA[:, 0:128], bd[:, j, :], xc[:, :])
                MM(psA[:, 128:256], bd[:, 4 + j, :], xc[:, :])
                yield
                ACT(rg[:, :], psA[:, 0:128], AF.Sigmoid, bias=ppc("ba", j))
                ACT(ig[:, :], psA[:, 128:256], AF.Sigmoid, bias=ppc("bi", j))
                yield
                ACT(aa[:, :], rg[:, :], AF.Exp, scale=cA[:, j:j + 1])
                TT("pool", bx[:, :], ig[:, :], xc[:, :], mult)
                yield
                TT("pool", a2[:, :], aa[:, :], aa[:, :], mult)
                yield
                ACT(a2[:, :], a2[:, :], AF.Sqrt, scale=-1.0, bias=1.0)
                yield
                TT("pool", bx[:, :], bx[:, :], a2[:, :], mult)
                yield
                SCAN(hs[:, :], aa[:, :], bx[:, :], hprev[:, j:j + 1], mult, add)
                ACT(gl[:, :], pG[j][:, :], AF.Gelu_apprx_tanh)
                yield
                CP("pool", hprev[:, j:j + 1], hs[:, 127:128])
                TT("pool", yy[:, :], hs[:, :], gl[:, :], mult)
                yield
                TT("pool", y2[:, :], yy[:, :], yy[:, :], mult)
                yield
                psN = psf()
                MM(psN[:, 0:128], bones, y2[:, :])
                yield
                ACT(rn[:, :], psN[:, 0:128], AF.Sqrt, bias=RMS_EPS)
                yield
                RECIP(rn[:, :], rn[:, :])
                yield
                STT(yTp[:, j, :], yy[:, :], ppc("lg", j), rn[:, :], mult, mult)
                yield

            lockstep([lru_chain(j) for j in range(4)])
            if stop <= 3:
                continue
            for m in range(14):
                src = pR[m]
                dd = tmpj(13, m % 4)
                TT("pool", dd[:, :], src[:, HALO - 1:HALO + 127], src[:, HALO:HALO + 128], sub)
                STT(us[m][:, :], dd[:, :], ppc("mu", m), src[:, HALO:HALO + 128], mult, add)
            ACT(us[12][0:64, :], us[12][0:64, :], AF.Tanh)
            ACT(us[13][:, :], us[13][:, :], AF.Sigmoid)

            def rwkv_chain(j):
                (sgw, cs, eL, eLn, eLm, eLc, av, kk0, sqk, kkn, tq, k2, bt) = (tmpj(s_, j) for s_ in range(13))
                csm, nrm, rk = sgw, sqk, kk0
                psL = psf()
                MM(psL[:, 0:128], lup[:, 0, j * 128:(j + 1) * 128], us[12][:, :])
                MM(psL[:, 128:256], lup[:, 1, j * 128:(j + 1) * 128], us[12][:, :])
                MM(psL[:, 256:384], lup[:, 2, j * 128:(j + 1) * 128], us[13][:, :])
                yield
                ACT(sgw[:, :], psL[:, 0:128], AF.Sigmoid, bias=ppc("w0", j))
                ACT(av[:, :], psL[:, 128:256], AF.Sigmoid, bias=ppc("a0", j))
                ACT(gT[:, j, :], psL[:, 256:384], AF.Identity)
                TS("dve", kk0[:, :], us[4 + j][:, :], ppc("kk", j), mult)
                yield
                SCAN(cs[:, :], ones, sgw[:, :], 0.0, mult, add)
                TT("pool", sqk[:, :], kk0[:, :], kk0[:, :], mult)
                yield
                psK = psf()
                MM(psK[:, 0:128], bones, sqk[:, :])
                ACT(eL[:, :], cs[:, :], AF.Exp, scale=-LDK)
                TS("dve", tq[:, :], av[:, :], ppc("ka", j), mult, omka[:, j:j + 1], add)
                yield
                ACT(eLn[:, :], cs[:, :], AF.Exp, scale=LDK)
                TT("pool", csm[:, :], cs[:, :], sgw[:, :], sub)
                TS("dve", nbt[:, j:j + 1], cs[:, 127:128], -LDK, mult)
                yield
                ACT(eLm[:, :], csm[:, :], AF.Exp, scale=-LDK)
                CP("pool", WC[:, j:j + 1], eL[:, 127:128])
                TT("pool", k2[:, :], us[4 + j][:, :], tq[:, :], mult)
                yield
                ACT(eLc[:, :], cs[:, :], AF.Exp, scale=LDK, bias=nbt[:, j:j + 1])
                yield
                ACT(nrm[:, :], psK[:, 0:128], AF.Sqrt, scale=64.0)
                yield
                TS("dve", nrm[:, :], nrm[:, :], 1e-12, ALU.max)
                yield
                RECIP(nrm[:, :], nrm[:, :])
                yield
                TT("pool", kkn[:, :], kk0[:, :], nrm[:, :], mult)
                CP("dve", vTb[:, j, :], us[8 + j][:, :])
                yield
                TT("pool", bt[:, :], kkn[:, :], av[:, :], mult)
                for hp in range(2):
                    hs_ = slice(hp * 64, hp * 64 + 64)
                    STT(ARz[hp][hs_, j, 0, :], kkn[hs_, :], -1.0, eLm[hs_, :], mult, mult)
                yield
                for hp in range(2):
                    hs_ = slice(hp * 64, hp * 64 + 64)
                    TT("pool", ARz[hp][hs_, j, 1, :], us[j][hs_, :], eL[hs_, :], mult)
                STT(rk[:, :], us[j][:, :], ppc("rk", j), k2[:, :], mult, mult)
                yield
                TT("pool", BK[:, j, 0, :], bt[:, :], eLn[:, :], mult)
                psBn = psf()
                MM(psBn[:, 0:128], bones, rk[:, :])
                yield
                TT("pool", BK[:, j, 1, :], k2[:, :], eLn[:, :], mult)
                STT(bon[:, j, :], psBn[:, 0:128], 64.0, us[8 + j][:, :], mult, mult)
                yield
                TT("pool", BKp[:, j, 0, :], bt[:, :], eLc[:, :], mult)
                yield
                TT("pool", BKp[:, j, 1, :], k2[:, :], eLc[:, :], mult)
                yield

            lockstep([rwkv_chain(j) for j in range(4)])
            if stop <= 4:
                continue
            for which, dst in ((None, Vt), (0, Bt), (1, Kt)):
                pb = psb()
                for j in range(4):
                    srcv = vTb[:, j, :] if which is None else BKp[:, j, which, :]
                    TR(pb[:, j * 128:(j + 1) * 128], srcv, identb[:, :])
                CP("act" if which is None else "dve", dst[:, :], pb[:, 0:512])
            cur, nxt = STb[blk % 2], STb[(blk + 1) % 2]

            def hd(g, i):
                h = 4 * g + i
                return h, h // 2, slice((h % 2) * 64, (h % 2) * 64 + 64)

            for g in range(2):
                for i in range(4):
                    h, j, psl = hd(g, i)
                    hp = h % 2
                    psAb = psf()
                    MM(psAb[:, 0:256], BK[:, j, 0, :], ARz[hp][:, j, :, :])
                    TT("dve", M1[g][:, i, 0:256], psAb[:, 0:256], maskSI, mult)
                    psAk = psf()
                    MM(psAk[:, 0:256], BK[:, j, 1, :], ARz[hp][:, j, :, :])
                    TT("dve", M1[g][:, i, 256:512], psAk[:, 0:256], maskSI, mult)
                pbt = psb()
                for i in range(4):
                    TR(pbt[:, i * 128:(i + 1) * 128], M1[g][:, i, 0:128], identb[:, :])
                CP("act", NTb[g][0][:, :], pbt[:, 0:512])
                for i in range(4):
                    TT("pool", Zb[g][0][:, i * 128:(i + 1) * 128], M1[g][:, i, 0:128], identb[:, :], add)
            if stop <= 5:
                continue
            Ncur = [[M1[g][:, i, 0:128] for i in range(4)] for g in range(2)]
            NTcur = [[NTb[g][0][:, i * 128:(i + 1) * 128] for i in range(4)] for g in range(2)]
            Zcur = [Zb[g][0] for g in range(2)]
            for lv in range(1, 7):
                pq = lv % 2
                psNTn = [psf() for g in range(2)]
                for g in range(2):
                    for i in range(4):
                        MM(psNTn[g][:, i * 128:(i + 1) * 128], Ncur[g][i], NTcur[g][i])
                if lv < 6:
                    psNn = [psf() for g in range(2)]
                    for g in range(2):
                        for i in range(4):
                            MM(psNn[g][:, i * 128:(i + 1) * 128], NTcur[g][i], Ncur[g][i])
                for g in range(2):
                    CP("act", NTb[g][pq][:, :], psNTn[g][:, 0:512])
                if lv < 6:
                    for g in range(2):
                        CP("dve", Nb[g][pq][:, :], psNn[g][:, 0:512])
                NTn = [[NTb[g][pq][:, i * 128:(i + 1) * 128] for i in range(4)] for g in range(2)]
                psZ = [psf() for g in range(2)]
                for g in range(2):
                    for i in range(4):
                        zc = Zcur[g][:, i * 128:(i + 1) * 128]
                        MM(psZ[g][:, i * 128:(i + 1) * 128], identb[:, :], zc, start=True, stop=False)
                        MM(psZ[g][:, i * 128:(i + 1) * 128], NTn[g][i], zc, start=False, stop=True)
                for g in range(2):
                    CP("act" if g == 0 else "dve", Zb[g][pq][:, :], psZ[g][:, 0:512])
                Zcur = [Zb[g][pq] for g in range(2)]
                NTcur = NTn
                if lv < 6:
                    Ncur = [[Nb[g][pq][:, i * 128:(i + 1) * 128] for i in range(4)] for g in range(2)]
            if stop <= 6:
                continue
            psX = [psf() for g in range(2)]
            for g in range(2):
                for i in range(4):
                    h, j, psl = hd(g, i)
                    MM(psX[g][:, i * 64:(i + 1) * 64], M1[g][:, i, 256:384], Vt[:, h * 64:(h + 1) * 64], start=True, stop=False)
                    MM(psX[g][:, i * 64:(i + 1) * 64], ARz[h % 2][:, j, 0, :], cur[:, j, :], start=False, stop=True)
            for g in range(2):
                CP("act" if g == 0 else "dve", Xb[:, g, :], psX[g][:, 0:256])
            psU = [psf() for g in range(2)]
            for g in range(2):
                for i in range(4):
                    MM(psU[g][:, i * 64:(i + 1) * 64], Zcur[g][:, i * 128:(i + 1) * 128], Xb[:, g, i * 64:(i + 1) * 64])
            for g in range(2):
                CP("act" if g == 0 else "dve", Ub[:, g, :], psU[g][:, 0:256])
            psO = [psf() for g in range(2)]
            psS = psf()
            for g in range(2):
                for i in range(4):
                    h, j, psl = hd(g, i)
                    ip = (i // 2) * 2
                    ubp = Ub[:, g, ip * 64:ip * 64 + 128]
                    vvp = Vt[:, j * 128:(j + 1) * 128]
                    ub = Ub[:, g, i * 64:(i + 1) * 64]
                    vv = Vt[:, h * 64:(h + 1) * 64]
                    oc = slice(i * 128, (i + 1) * 128)
                    MM(psO[g][:, oc], ubp, M1[g][:, i, 128:256], start=True, stop=False)
                    MM(psO[g][:, oc], vvp, M1[g][:, i, 384:512], start=False, stop=False)
                    MM(psO[g][psl, oc], cur[psl, j, :], ARz[h % 2][psl, j, 1, :], start=False, stop=True)
                    MM(psS[:, h * 64:(h + 1) * 64], Bt[:, j * 128:(j + 1) * 128], ub, start=True, stop=False)
                    MM(psS[:, h * 64:(h + 1) * 64], Kt[:, j * 128:(j + 1) * 128], vv, start=False, stop=True)
            for h in range(8):
                j, psl = h // 2, slice((h % 2) * 64, (h % 2) * 64 + 64)
                STT(STf[psl, j, :], STf[psl, j, :], WC[psl, j:j + 1], psS[psl, h * 64:(h + 1) * 64], mult, add)
            CP("pool", nxt[:, :, :], STf[:, :, :])
            for g in range(2):
                for i in range(4):
                    h, j, psl = hd(g, i)
                    CP("act" if i % 2 == 0 else "dve", OT[psl, j * 128:(j + 1) * 128], psO[g][psl, i * 128:(i + 1) * 128])
            if stop <= 7:
                continue
            def post_chain(j):
                cen, sq2, sd, on = (tmpj(s_, j) for s_ in range(4))
                psM = psf()
                MM(psM[:, 0:128], bones, OT[:, j * 128:(j + 1) * 128])
                yield
                TT("dve", cen[:, :], OT[:, j * 128:(j + 1) * 128], psM[:, 0:128], sub)
                yield
                TT("pool", sq2[:, :], cen[:, :], cen[:, :], mult)
                yield
                psV = psf()
                MM(psV[:, 0:128], bones, sq2[:, :])
                yield
                ACT(sd[:, :], psV[:, 0:128], AF.Sqrt, bias=GN_EPS)
                yield
                RECIP(sd[:, :], sd[:, :])
                yield
                TT("pool", on[:, :], cen[:, :], sd[:, :], mult)
                yield
                TS("dve", on[:, :], on[:, :], ppc("lnw", j), mult, ppc("lnb", j), add)
                yield
                TT("pool", on[:, :], on[:, :], bon[:, j, :], add)
                yield
                TT("pool", yTp[:, 4 + j, :], on[:, :], gT[:, j, :], mult)
                yield

            lockstep([post_chain(j) for j in range(4)])
            if stop <= 8:
                continue
            if blk >= NBLK // 2:
                tok0 = (blk - NBLK // 2) * 128
                DMA("sp", "x", xr[par][:, :], xs[r0:r0 + 128, :])
                for nh in range(2):
                    ps = psf()
                    cs_ = slice(nh * 512, (nh + 1) * 512)
                    for kc in range(8):
                        MM(ps[:, 0:512], yTp[:, kc, :], w_out_sb[:, kc, cs_], start=(kc == 0), stop=(kc == 7))
                    TT("dve", xo[par][:, cs_], ps[:, 0:512], g1b[:, cs_], mult)
                    TT("pool", xo[par][:, cs_], xo[par][:, cs_], xr[par][:, cs_], add)
                DMA("sp", "xn", xnew[tok0:tok0 + 128, :], xo[par][:, :])
        P.barrier()
        es_m.close()
        self.moe(es, es_all, xnew, outd, w1d, w3d, w2d, s2T, modT, g2b, bcB, wr, cst, identb)
        fin = [(k, P.count[k]) for k in P.semkeys]
        P.wait_all("sp", fin)
        P.emit()
        es.close()
        es_all.close()
        return nc

    def moe(self, es, es_all, xnew, outd, w1d, w3d, w2d, s2T, modT, g2b, bcB, wr, cst, identb):
        nc, P, NOWN = self.nc, self.P, self.NOWN
        ACT, TS, STT, TT, CP, MM, TR, DMA, RECIP, MSET = (self.ACT, self.TS, self.STT, self.TT, self.CP, self.MM,
                                                          self.TR, self.DMA, self.RECIP, self.MSET)
        mult, add, sub = ALU.mult, ALU.add, ALU.subtract
        sb, psf = self.sb, self.psf
        es_e = ExitStack()
        NTT = NOWN // 128
        fgb = bcB[:, 0:D]
        xl = [sb(es_e, "xl%d" % i, [128, D], F32) for i in range(2)]
        ot = [sb(es_e, "ot%d" % i, [128, D], F32) for i in range(2)]
        sqj = sb(es_e, "sqj2", [128, D], BF16)
        ss = sb(es_e, "ss2", [128, 2], F32)
        rs = sb(es_e, "rs2", [128, 2], F32)
        if self.debug == "nomoe":
            for tt in range(NTT):
                par = tt % 2
                DMA("sp", "x", xl[par][:, :], xnew[tt * 128:(tt + 1) * 128, :])
                ACT(sqj[:, :], xl[par][:, :], AF.Square, accum=ss[:, par:par + 1])
                ACT(rs[:, par:par + 1], ss[:, par:par + 1], AF.Sqrt, scale=1.0 / D, bias=RMS_EPS)
                RECIP(rs[:, par:par + 1], rs[:, par:par + 1])
                STT(ot[par][:, :], xl[par][:, :], rs[:, par:par + 1], fgb, mult, mult)
                DMA("sp", "o", outd[tt * 128:(tt + 1) * 128, :], ot[par][:, :])
            es_e.close()
            return
        es_e.close()
        RED, MAX8 = self.RED, self.MAX8
        ident = cst[:, C_ID:C_ID + 128]
        brow = bcB[:, D:D + 36]
        NH = 2 if NOWN >= 2048 else 1
        HT = NOWN // NH
        NT_ = HT // 128
        TBK = min(512, HT)
        es_h = ExitStack()
        h2T = sb(es_h, "h2T", [128, 8, HT], BF16)
        acc = [sb(es_h, "acc%d" % i, [128, D], F32) for i in range(NT_)]
        gate = sb(es_h, "gate", [128, NT_, NE], F32)
        for hf in range(NH):
            t0 = hf * HT
            es1 = ExitStack()
            xl = [sb(es1, "m1xl%d" % i, [128, D], F32) for i in range(2)]
            xn2 = [sb(es1, "m1xn%d" % i, [128, D], F32) for i in range(2)]
            h2f = sb(es1, "h2f", [128, 8, 128], F32)
            sqj = sb(es1, "m1sq", [128, D], BF16)
            sm = {n: sb(es1, "m1_" + n, [128, w], F32) for n, w in (
                ("ss", 2), ("rs", 2), ("lg", 36), ("gmax", 1), ("ngmax", 1), ("e4", 4), ("gsum", 1), ("oh4", 4),
                ("ig8", 8), ("m8", 8), ("sel", 8), ("nv1", 1), ("e8", 8), ("w8", 8), ("den", 1), ("coef", 1))}
            for tt in range(NT_):
                par = tt % 2
                r0 = t0 + tt * 128
                DMA("sp", "x", xl[par][:, :], xnew[r0:r0 + 128, :])
                ACT(sqj[:, :], xl[par][:, :], AF.Square, accum=sm["ss"][:, par:par + 1])
                ACT(sm["rs"][:, par:par + 1], sm["ss"][:, par:par + 1], AF.Sqrt, scale=1.0 / D, bias=RMS_EPS)
                RECIP(sm["rs"][:, par:par + 1], sm["rs"][:, par:par + 1])
                TS("dve", xn2[par][:, :], xl[par][:, :], sm["rs"][:, par:par + 1], mult)
                if self.mstop <= 0.1:
                    continue
                pst = [psf(), psf()]
                for kc in range(8):
                    TR(pst[kc // 4][:, (kc % 4) * 128:(kc % 4 + 1) * 128], xn2[par][:, kc * 128:(kc + 1) * 128], ident)
                if self.mstop <= 0.2:
                    continue
                for kc in range(8):
                    pv = pst[kc // 4][:, (kc % 4) * 128:(kc % 4 + 1) * 128]
                    ACT(h2T[:, kc, tt * 128:(tt + 1) * 128], pv, AF.Identity, scale=s2T[:, kc:kc + 1],
                        bias=modT[:, 16 + kc:17 + kc])
                    TS("dve", h2f[:, kc, :], pv, s2T[:, kc:kc + 1], mult, modT[:, 16 + kc:17 + kc], add)
                if self.mstop <= 0.5:
                    continue
                psR = psf()
                for kc in range(8):
                    MM(psR[:, 0:36], h2f[:, kc, :], wr[:, kc, :], start=(kc == 0), stop=(kc == 7))
                lg = sm["lg"]
                TT("dve", lg[:, :], psR[:, 0:36], brow, add)
                RED(sm["gmax"][:, :], lg[:, 0:4], ALU.max)
                TS("dve", sm["ngmax"][:, :], sm["gmax"][:, :], -1.0, mult)
                ACT(sm["e4"][:, :], lg[:, 0:4], AF.Exp, bias=sm["ngmax"][:, 0:1], accum=sm["gsum"][:, 0:1])
                TS("dve", sm["oh4"][:, :], lg[:, 0:4], sm["gmax"][:, 0:1], ALU.is_ge)
                TS("dve", sm["ig8"][:, :], lg[:, 4:12], sm["oh4"][:, 0:1], mult)
                for g in range(1, 4):
                    STT(sm["ig8"][:, :], lg[:, 4 + 8 * g:12 + 8 * g], sm["oh4"][:, g:g + 1], sm["ig8"][:, :], mult, add)
                MAX8(sm["m8"][:, :], sm["ig8"][:, :])
                TS("dve", sm["sel"][:, :], sm["ig8"][:, :], sm["m8"][:, 1:2], ALU.is_ge)
                TS("dve", sm["nv1"][:, :], sm["m8"][:, 0:1], -1.0, mult)
                ACT(sm["e8"][:, :], sm["ig8"][:, :], AF.Exp, bias=sm["nv1"][:, 0:1])
                TT("dve", sm["w8"][:, :], sm["e8"][:, :], sm["sel"][:, :], mult)
                RED(sm["den"][:, :], sm["w8"][:, :], ALU.add)
                TT("dve", sm["coef"][:, :], sm["den"][:, :], sm["gsum"][:, :], mult)
                RECIP(sm["coef"][:, :], sm["coef"][:, :])
                TS("dve", sm["w8"][:, :], sm["w8"][:, :], sm["coef"][:, 0:1], mult)
                for g in range(4):
                    TS("dve", gate[:, tt, 8 * g:8 * g + 8], sm["w8"][:, :], sm["oh4"][:, g:g + 1], mult)
                MSET("pool", acc[tt][:, :], 0.0)
            P.barrier()
            es1.close()
            es2 = ExitStack()
            w1b = [sb(es2, "w1b%d" % i, [128, 8, DE], BF16) for i in range(2)]
            w3b = [sb(es2, "w3b%d" % i, [128, 8, DE], BF16) for i in range(2)]
            w2b = [sb(es2, "w2b%d" % i, [128, 4, D], BF16) for i in range(2)]
            hid = [sb(es2, "hid%d" % i, [128, 4, TBK], BF16) for i in range(2)]
            s1t = [sb(es2, "s1t%d" % i, [128, TBK], F32) for i in range(2)]
            cnt = 0
            for e in range(self.ne if self.mstop > 1 else 0):
                wb = e % 2
                DMA("pool", "wm", w1b[wb][:, :, :], V(w1d, w1d.t[e].rearrange("(k p) n -> p k n", p=128)))
                DMA("pool", "wm", w3b[wb][:, :, :], V(w3d, w3d.t[e].rearrange("(k p) n -> p k n", p=128)))
                DMA("pool", "wm", w2b[wb][:, :, :], V(w2d, w2d.t[e].rearrange("(k p) n -> p k n", p=128)))
                for tb in range(HT // TBK if self.mstop > 2 else 0):
                    hb = hid[cnt % 2]
                    cnt += 1
                    tcs = slice(tb * TBK, (tb + 1) * TBK)
                    for hc in range(4):
                        ps1 = psf()
                        for kc in range(8):
                            MM(ps1[:, 0:TBK], w1b[wb][:, kc, hc * 128:(hc + 1) * 128], h2T[:, kc, tcs],
                               start=(kc == 0), stop=(kc == 7))
                        ps3 = psf()
                        for kc in range(8):
                            MM(ps3[:, 0:TBK], w3b[wb][:, kc, hc * 128:(hc + 1) * 128], h2T[:, kc, tcs],
                               start=(kc == 0), stop=(kc == 7))
                        st_ = s1t[hc % 2]
                        ACT(st_[:, :], ps1[:, 0:TBK], AF.Silu)
                        TT("dve", hb[:, hc, :], st_[:, :], ps3[:, 0:TBK], mult)
                    for t4 in range(TBK // 128 if self.mstop > 3 else 0):
                        tt = tb * (TBK // 128) + t4
                        for nh in range(2):
                            cs_ = slice(nh * 512, (nh + 1) * 512)
                            psY = psf()
                            for hc in range(4):
                                MM(psY[:, 0:512], hb[:, hc, t4 * 128:(t4 + 1) * 128], w2b[wb][:, hc, cs_],
                                   start=(hc == 0), stop=(hc == 3))
                            STT(acc[tt][:, cs_], psY[:, 0:512], gate[:, tt, e:e + 1], acc[tt][:, cs_], mult, add)
            P.barrier()
            es2.close()
            es3 = ExitStack()
            xl = [sb(es3, "m3xl%d" % i, [128, D], F32) for i in range(2)]
            ot = [sb(es3, "m3ot%d" % i, [128, D], F32) for i in range(2)]
            sqj = sb(es3, "m3sq", [128, D], BF16)
            ss = sb(es3, "m3ss", [128, 2], F32)
            rs = sb(es3, "m3rs", [128, 2], F32)
            for tt in range(NT_):
                par = tt % 2
                r0 = t0 + tt * 128
                DMA("sp", "x", xl[par][:, :], xnew[r0:r0 + 128, :])
                TT("dve", acc[tt][:, :], acc[tt][:, :], g2b[:, :], mult)
                TT("pool", xl[par][:, :], xl[par][:, :], acc[tt][:, :], add)
                ACT(sqj[:, :], xl[par][:, :], AF.Square, accum=ss[:, par:par + 1])
                ACT(rs[:, par:par + 1], ss[:, par:par + 1], AF.Sqrt, scale=1.0 / D, bias=RMS_EPS)
                RECIP(rs[:, par:par + 1], rs[:, par:par + 1])
                STT(ot[par][:, :], xl[par][:, :], rs[:, par:par + 1], fgb, mult, mult)
                DMA("sp", "o", outd[r0:r0 + 128, :], ot[par][:, :])
            P.barrier()
            es3.close()
        es_h.close()


def _cols(v, n):
    return np.ascontiguousarray(np.asarray(v, np.float32).reshape(n, 128).T)


def _host_consts():
    c = np.zeros((128, NCONST), np.float32)
    c[:, C_ID:C_ID + 128] = np.eye(128, dtype=np.float32)
    bo = np.zeros((128, 128), np.float32)
    bo[:64, :64] = 1.0 / 64
    bo[64:, 64:] = 1.0 / 64
    c[:, C_BONES:C_BONES + 128] = bo
    s = np.arange(128)[:, None]
    t = np.arange(128)[None, :]
    c[:, C_TRIS:C_TRIS + 128] = (s < t)
    c[:, C_TRII:C_TRII + 128] = (s <= t)
    c[:, C_ONES:C_ONES + 128] = 1.0
    for i in range(4):
        c[:, C_TRIL4 + i * 128:C_TRIL4 + (i + 1) * 128] = (s > t)
    return c


def _prep_inputs(inputs, T, with_moe=True, ne=NE):
    f = lambda k: np.asarray(inputs[k], np.float32)
    x = f("x")
    B = x.shape[0]
    half = T // 2
    pp = np.zeros((128, NPP), np.float32)

    def put(name, v, n):
        pp[:, PP[name]:PP[name] + n] = _cols(v, n)

    b_ada = f("b_ada")[0]
    put("n1g", f("norm1_g")[0], 8)
    put("n2g", f("norm2_g")[0], 8)
    put("bsh1", b_ada[0:1024], 8)
    put("bsc1", b_ada[1024:2048], 8)
    put("bsh2", b_ada[3072:4096], 8)
    put("bsc2", b_ada[4096:5120], 8)
    cw = f("conv_w")[0]
    for tap in range(4):
        pp[:, PP["cw"] + tap * 4:PP["cw"] + tap * 4 + 4] = _cols(cw[tap], 4)
    put("cb", f("conv_b")[0], 4)
    put("ba", f("lru_ba")[0].reshape(-1), 4)
    put("bi", f("lru_bi")[0].reshape(-1), 4)
    put("lam", f("lru_lam")[0], 4)
    put("lg", f("lru_norm_g")[0], 4)
    put("mu", f("tok_mu")[0], 14)
    put("w0", f("w0")[0], 4)
    put("a0", f("a0")[0], 4)
    put("kk", f("k_k")[0], 4)
    put("ka", f("k_a")[0], 4)
    put("rk", f("r_k")[0].reshape(-1), 4)
    put("lnw", f("ln_x_w")[0], 4)
    put("lnb", f("ln_x_b")[0], 4)
    wa, wi = f("lru_wa")[0], f("lru_wi")[0]
    bd = np.zeros((128, 8, 128), np.float32)
    for j in range(4):
        for hp in range(2):
            sl = slice(hp * 64, hp * 64 + 64)
            bd[sl, j, sl] = wa[2 * j + hp]
            bd[sl, 4 + j, sl] = wi[2 * j + hp]
    lup = np.zeros((128, 3, 512), np.float32)
    lup[0:64, 0] = f("w_up")[0]
    lup[64:128, 1] = f("a_up")[0]
    lup[:, 2] = f("g_up")[0]
    bcA = np.ascontiguousarray(np.broadcast_to(np.concatenate([b_ada[2048:3072], b_ada[5120:6144]])[None, :], (128, 2048)))
    bcB = np.ascontiguousarray(np.broadcast_to(
        np.concatenate([f("final_g"), f("b_grp")[0], f("b_exp")[0]])[None, :], (128, 1024 + 36)))
    wrr = np.concatenate([f("w_grp")[0], f("w_exp")[0]], axis=1)
    wr = np.ascontiguousarray(wrr.reshape(8, 128, 36).transpose(1, 0, 2))
    consts = _host_consts()
    shared = {"w_ada": f("w_ada")[0], "pp": pp, "w_in": f("w_in")[0], "w_out": f("w_out")[0], "bd": bd, "lup": lup,
              "bcA": bcA, "bcB": bcB, "wr": wr, "consts": consts}
    if with_moe:
        shared.update({"w1": f("w1")[0][:ne], "w3": f("w3")[0][:ne], "w2": f("w2")[0][:ne]})
    c = f("c")
    in_maps = []
    nblk = T // 128
    for core in range(2 * B):
        b, hf = core // 2, core % 2
        m = dict(shared)
        bmk = np.ones((128, nblk), np.float32)
        if hf == 0:
            xs = np.concatenate([np.zeros((half, D), np.float32), x[b, :half]], axis=0)
            bmk[:, :nblk // 2] = 0.0
        else:
            xs = x[b]
        m["xs"] = np.ascontiguousarray(xs)
        m["cT"] = _cols(c[b], 8)
        m["blkmask"] = bmk
        in_maps.append(m)
    return in_maps


_NC_CACHE = {}


def kernel(_debug=None, _stop=99, _ne=NE, _mstop=99, **inputs):
    x = np.asarray(inputs["x"])
    B, T, _ = x.shape
    in_maps = _prep_inputs(inputs, T, with_moe=(_debug != "nomoe"), ne=_ne)
    key = (T, _debug, _stop, _ne, _mstop)
    if key not in _NC_CACHE:
        _NC_CACHE[key] = KB(T, debug=_debug, stop=_stop, ne=_ne, mstop=_mstop).build()
    nc = _NC_CACHE[key]
    res = run_bass_kernel_spmd(nc, in_maps, core_ids=list(range(2 * B)))
    out = np.zeros((B, T, D), np.float32)
    half = T // 2
    for core in range(2 * B):
        b, hf = core // 2, core % 2
        out[b, hf * half:(hf + 1) * half] = res.results[core]["out"]
    return out
```

```python
import numpy as np
from contextlib import ExitStack
import concourse.bass as bass
import concourse.mybir as mybir
from concourse.bass_utils import run_bass_kernel_spmd

F32 = mybir.dt.float32
BF16 = mybir.dt.bfloat16
U32 = mybir.dt.uint32
AF = mybir.ActivationFunctionType
ALU = mybir.AluOpType
AX = mybir.AxisListType

D = 1024
SEQ = 8192
NB = 4
PROJ = 2816
NCH = 22
CH = 128
NE = 32
DE = 512
RMS_EPS = 1e-6
GN_EPS = 64e-5


class Tl:
    def __init__(self, t, name=""):
        self.t = t
        self.name = name
        self.w = None
        self.r = []
        self.wstream = None
        self.psum = name.startswith("ps")

    def __getitem__(self, idx):
        return V(self, self.t[idx])


class V:
    __slots__ = ("tl", "ap")

    def __init__(self, tl, ap):
        self.tl = tl
        self.ap = ap


class Prog:
    STREAMS = ("pe", "act", "dve", "pool", "sp")

    def __init__(self, nc):
        self.nc = nc
        self.ops = {s: [] for s in self.STREAMS}
        self.count = {}
        self.seen = {s: {} for s in self.STREAMS}
        self.semkeys = []

    def _tok(self, key, amt):
        self.count[key] = self.count.get(key, 0) + amt
        if key not in self.semkeys:
            self.semkeys.append(key)
        return (key, self.count[key])

    def op(self, stream, fn, reads=(), writes=(), dma=None):
        waits = {}

        def need(tok, tstream):
            if tok is None:
                return
            key, val = tok
            if tstream == stream and stream == "pe":
                return
            if self.seen[stream].get(key, 0) >= val:
                return
            if waits.get(key, 0) < val:
                waits[key] = val

        for tl in reads:
            need(tl.w, tl.wstream)
            if tl.psum:
                for (k, v, s) in tl.r:
                    if s != stream:
                        need((k, v), s)
        for tl in writes:
            need(tl.w, tl.wstream)
            for (k, v, s) in tl.r:
                if s == stream and dma is None:
                    continue
                need((k, v), s)
        for k, v in waits.items():
            self.seen[stream][k] = v
        if dma is not None:
            tok = self._tok("q_" + dma, 16)
            tstream = "dma"
        else:
            tok = self._tok("s_" + stream, 1)
            tstream = stream
        self.ops[stream].append((list(waits.items()), fn, tok[0], 16 if dma is not None else 1))
        for tl in reads:
            tl.r.append((tok[0], tok[1], tstream))
        for tl in writes:
            tl.w = tok
            tl.wstream = tstream
            tl.r = []
        return tok

    def wait_all(self, stream, toks):
        waits = {}
        for key, val in toks:
            if self.seen[stream].get(key, 0) < val and waits.get(key, 0) < val:
                waits[key] = val
        for k, v in waits.items():
            self.seen[stream][k] = v
        self.ops[stream].append((list(waits.items()), None, None, 0))

    def emit(self):
        nc = self.nc
        with ExitStack() as es:
            sems = {k: es.enter_context(nc.semaphore(k)) for k in self.semkeys}
            block = es.enter_context(nc.Block())

            def replay(stream, eng):
                for waits, fn, key, amt in self.ops[stream]:
                    for k, v in waits:
                        eng.wait_ge(sems[k], v)
                    if fn is not None:
                        fn(eng).then_inc(sems[key], amt)

            @block.tensor
            def _(e):
                replay("pe", e)

            @block.scalar
            def _(e):
                replay("act", e)

            @block.vector
            def _(e):
                replay("dve", e)

            @block.gpsimd
            def _(e):
                replay("pool", e)

            @block.sync
            def _(e):
                replay("sp", e)

    def barrier(self):
        toks = [(k, self.count[k]) for k in self.semkeys]
        for s in self.STREAMS:
            self.wait_all(s, toks)


def _tls(*vs):
    return [v.tl for v in vs if isinstance(v, V)]


def _ap(v):
    return v.ap if isinstance(v, V) else v


_PP_ITEMS = [("n1g", 8), ("n2g", 8), ("bsh1", 8), ("bsc1", 8), ("bsh2", 8), ("bsc2", 8), ("cw", 16),
             ("cb", 4), ("ba", 4), ("bi", 4), ("lam", 4), ("lg", 4), ("mu", 14), ("w0", 4), ("a0", 4),
             ("kk", 4), ("ka", 4), ("rk", 4), ("lnw", 4), ("lnb", 4)]
PP = {}
_o = 0
for _n, _c in _PP_ITEMS:
    PP[_n] = _o
    _o += _c
NPP = _o
C_ID, C_BONES, C_TRIS, C_TRII, C_ONES, C_TRIL4, C_IOTA = 0, 128, 256, 384, 512, 640, 1152
NCONST = 1408
CAP = 256
HALO = 4
LDK = 0.6065306597126334


class KB:
    def __init__(self, T, debug=None, stop=99, ne=NE, mstop=99):
        self.T = T
        self.ne = ne
        self.mstop = mstop
        self.stop = stop
        self.NBLK = T // 128
        self.NOWN = T // 2
        self.debug = debug
        self.nc = bass.Bass("TRN2", target_bir_lowering=False)
        self.P = Prog(self.nc)
        self._nf = 0
        self._nb = 0
        self._dram_names = set()

    def sb(self, es, name, shape, dt):
        self._uid = getattr(self, "_uid", 0) + 1
        return Tl(es.enter_context(self.nc.sbuf_tensor("%s_u%d" % (name, self._uid), shape, dt)), name)

    def dram(self, name, shape, dt, kind):
        self._dram_names.add(name)
        return Tl(self.nc.dram_tensor(name, shape, dt, kind=kind).ap(), name)

    def psf(self):
        t = self.psF[self._nf % len(self.psF)]
        self._nf += 1
        return t

    def psb(self):
        t = self.psB[self._nb % len(self.psB)]
        self._nb += 1
        return t

    def ACT(self, out, in_, func, scale=None, bias=None, accum=None):
        kw = {}
        if scale is not None:
            kw["scale"] = _ap(scale)
        if bias is not None:
            kw["bias"] = _ap(bias)
        if accum is not None:
            kw["accum_out"] = accum.ap
        o, i = out.ap, in_.ap
        return self.P.op("act", lambda e: e.activation(out=o, in_=i, func=func, **kw),
                         _tls(in_, scale, bias), _tls(out, accum))

    def TS(self, eng, out, in0, s1, op0, s2=None, op1=None):
        o, i, a1, a2 = out.ap, in0.ap, _ap(s1), _ap(s2)
        kw = {} if op1 is None else {"op1": op1}
        return self.P.op(eng, lambda e: e.tensor_scalar(out=o, in0=i, scalar1=a1, scalar2=a2, op0=op0, **kw),
                         _tls(in0, s1, s2), _tls(out))

    def STT(self, out, in0, sc, in1, op0, op1):
        o, i0, s, i1 = out.ap, in0.ap, _ap(sc), in1.ap
        return self.P.op("dve", lambda e: e.scalar_tensor_tensor(out=o, in0=i0, scalar=s, in1=i1, op0=op0, op1=op1),
                         _tls(in0, sc, in1), _tls(out))

    def TT(self, eng, out, in0, in1, op):
        o, i0, i1 = out.ap, in0.ap, in1.ap
        return self.P.op(eng, lambda e: e.tensor_tensor(out=o, in0=i0, in1=i1, op=op), _tls(in0, in1), _tls(out))

    def CP(self, eng, out, in_):
        o, i = out.ap, in_.ap
        if eng == "act":
            return self.P.op("act", lambda e: e.copy(out=o, in_=i), _tls(in_), _tls(out))
        return self.P.op(eng, lambda e: e.tensor_copy(out=o, in_=i), _tls(in_), _tls(out))

    def MSET(self, eng, out, val):
        o = out.ap
        return self.P.op(eng, lambda e: e.memset(o, val), [], _tls(out))

    def RECIP(self, out, in_):
        o, i = out.ap, in_.ap
        return self.P.op("dve", lambda e: e.reciprocal(out=o, in_=i), _tls(in_), _tls(out))

    def MM(self, out, lhsT, rhs, start=True, stop=True):
        o, l, r = out.ap, lhsT.ap, rhs.ap
        return self.P.op("pe", lambda e: e.matmul(o, lhsT=l, rhs=r, start=start, stop=stop),
                         _tls(lhsT, rhs), _tls(out))

    def TR(self, out, in_, ident):
        o, i, d = out.ap, in_.ap, ident.ap
        return self.P.op("pe", lambda e: e.transpose(out=o, in_=i, identity=d), _tls(in_, ident), _tls(out))

    def DMA(self, stream, q, out, in_):
        o, i = out.ap, in_.ap
        side = in_.tl if out.tl.name in self._dram_names else out.tl
        return self.P.op(stream, lambda e: e.dma_start(out=o, in_=i), _tls(in_), _tls(out), dma=side.name)

    def RED(self, out, in_, op):
        o, i = out.ap, in_.ap
        return self.P.op("dve", lambda e: e.tensor_reduce(out=o, in_=i, axis=AX.X, op=op), _tls(in_), _tls(out))

    def MAX8(self, out, in_):
        o, i = out.ap, in_.ap
        return self.P.op("dve", lambda e: e.max(out=o, in_=i), _tls(in_), _tls(out))

    def SCAN(self, out, d0, d1, init, op0, op1):
        o, a, b, c = out.ap, d0.ap, d1.ap, _ap(init)
        return self.P.op("dve", lambda e: e.tensor_tensor_scan(out=o, data0=a, data1=b, initial=c, op0=op0, op1=op1),
                         _tls(d0, d1, init), _tls(out))

    def build(self):
        nc, P, T, NBLK, NOWN = self.nc, self.P, self.T, self.NBLK, self.NOWN
        ACT, TS, STT, TT, CP, MM, TR, DMA, SCAN, RECIP, MSET = (self.ACT, self.TS, self.STT, self.TT, self.CP,
                                                                self.MM, self.TR, self.DMA, self.SCAN, self.RECIP,
                                                                self.MSET)
        mult, add, sub = ALU.mult, ALU.add, ALU.subtract
        xs = self.dram("xs", [T, D], F32, "ExternalInput")
        cTd = self.dram("cT", [128, 8], F32, "ExternalInput")
        w_ada = self.dram("w_ada", [D, 6 * D], F32, "ExternalInput")
        ppd = self.dram("pp", [128, NPP], F32, "ExternalInput")
        w_in = self.dram("w_in", [D, PROJ], F32, "ExternalInput")
        w_out = self.dram("w_out", [D, D], F32, "ExternalInput")
        bdd = self.dram("bd", [128, 8, 128], F32, "ExternalInput")
        lupd = self.dram("lup", [128, 3, 512], F32, "ExternalInput")
        bcAd = self.dram("bcA", [128, 2048], F32, "ExternalInput")
        bcBd = self.dram("bcB", [128, 1024 + 36], F32, "ExternalInput")
        wrd = self.dram("wr", [128, 8, 36], F32, "ExternalInput")
        cstd = self.dram("consts", [128, NCONST], F32, "ExternalInput")
        bmd = self.dram("blkmask", [128, NBLK], F32, "ExternalInput")
        w1d = w3d = w2d = None
        if self.debug != "nomoe":
            w1d = self.dram("w1", [self.ne, D, DE], F32, "ExternalInput")
            w3d = self.dram("w3", [self.ne, D, DE], F32, "ExternalInput")
            w2d = self.dram("w2", [self.ne, DE, D], F32, "ExternalInput")
        xnew = self.dram("xnew", [NOWN, D], F32, "Internal")
        outd = self.dram("out", [NOWN, D], F32, "ExternalOutput")

        es_all = ExitStack()
        es = ExitStack()
        self.psF = [Tl(es_all.enter_context(nc.psum_tensor("psf%d" % i, [128, 512], F32)), "psf%d" % i) for i in range(6)]
        self.psB = [Tl(es_all.enter_context(nc.psum_tensor("psb%d" % i, [128, 1024], BF16)), "psb%d" % i) for i in range(2)]
        psf, psb = self.psf, self.psb
        sb = self.sb

        cst = sb(es, "cst", [128, NCONST], F32)
        pp = sb(es, "ppt", [128, NPP], F32)
        bcB = sb(es, "bcBt", [128, 1024 + 36], F32)
        wr = sb(es, "wrt", [128, 8, 36], F32)
        bm = sb(es, "bmt", [128, NBLK], F32)
        identb = sb(es, "identb", [128, 128], BF16)
        condT = sb(es, "condT", [128, 8], F32)
        modT = sb(es, "modT", [128, 32], F32)
        s1T = sb(es, "s1T", [128, 8], F32)
        s2T = sb(es, "s2T", [128, 8], F32)
        g2b = sb(es, "g2b", [128, D], F32)
        cA = sb(es, "cA", [128, 4], F32)
        omka = sb(es, "omka", [128, 4], F32)

        def ppc(name, i=0):
            return pp[:, PP[name] + i:PP[name] + i + 1]

        ident = cst[:, C_ID:C_ID + 128]
        bones = cst[:, C_BONES:C_BONES + 128]
        maskSI = cst[:, C_TRIS:C_TRIS + 256]
        ones = cst[:, C_ONES:C_ONES + 128]
        maskL4 = cst[:, C_TRIL4:C_TRIL4 + 512]

        DMA("sp", "c", cst[:, :], cstd[:, :])
        DMA("sp", "c", pp[:, :], ppd[:, :])
        DMA("sp", "c", condT[:, :], cTd[:, :])
        DMA("sp", "c", bcB[:, :], bcBd[:, :])
        DMA("sp", "c", wr[:, :, :], wrd[:, :, :])
        DMA("sp", "c", bm[:, :], bmd[:, :])
        es_m = ExitStack()
        bd = sb(es_m, "bdt", [128, 8, 128], F32)
        lup = sb(es_m, "lupt", [128, 3, 512], F32)
        g1b = sb(es_m, "g1b", [128, D], F32)
        cbm = sb(es_m, "cbm", [128, 4, NBLK], F32)
        DMA("sp", "c", bd[:, :, :], bdd[:, :, :])
        DMA("sp", "c", lup[:, :, :], lupd[:, :, :])
        w_in_sb = sb(es_m, "w_in_sb", [128, 8, PROJ], BF16)
        w_out_sb = sb(es_m, "w_out_sb", [128, 8, D], BF16)
        es_s = ExitStack()
        wst = [sb(es_s, "wst%d" % i, [128, 8, 512], F32) for i in range(2)]
        condbc = sb(es_s, "condbc", [128, 8, 128], F32)
        bcA = sb(es_s, "bcAt", [128, 2048], F32)
        DMA("sp", "c", bcA[:, :], bcAd[:, :])
        for kc in range(8):
            for hh in range(2):
                c0 = hh * 1408
                DMA("pool", "w", w_in_sb[:, kc, c0:c0 + 1408], w_in[kc * 128:(kc + 1) * 128, c0:c0 + 1408])
        for kc in range(8):
            DMA("pool", "w", w_out_sb[:, kc, :], w_out[kc * 128:(kc + 1) * 128, :])
        CP("dve", identb[:, :], ident)
        ACT(condT[:, :], condT[:, :], AF.Silu)
        for kc in range(8):
            TS("dve", condbc[:, kc, :], ones, condT[:, kc:kc + 1], mult)
        psMod = psf()
        fm = {0: 0, 1: 0, 2: 1, 3: 1, 6: 2, 7: 2, 8: 3, 9: 3}
        for gi in range(12):
            wb = wst[gi % 2]
            src = V(w_ada, w_ada.t[:, gi * 512:(gi + 1) * 512].rearrange("(k p) n -> p k n", p=128))
            DMA("sp", "c", wb[:, :, :], src)
            if gi in fm:
                for n4 in range(4):
                    col = fm[gi] * 8 + (gi % 2) * 4 + n4
                    for kc in range(8):
                        MM(psMod[:, col:col + 1], wb[:, kc, n4 * 128:(n4 + 1) * 128], condT[:, kc:kc + 1],
                           start=(kc == 0), stop=(kc == 7))
            else:
                ps = psf()
                for kc in range(8):
                    MM(ps[:, 0:512], condbc[:, kc, :], wb[:, kc, :], start=(kc == 0), stop=(kc == 7))
                dst = g1b if gi < 6 else g2b
                c0 = (gi % 2) * 512
                b0 = (0 if gi < 6 else 1024) + c0
                TT("dve", dst[:, c0:c0 + 512], ps[:, 0:512], bcA[:, b0:b0 + 512], add)
        TT("dve", modT[:, :], psMod[:, 0:32], pp[:, PP["bsh1"]:PP["bsh1"] + 32], add)
        STT(s1T[:, :], modT[:, 8:16], 1.0, pp[:, PP["n1g"]:PP["n1g"] + 8], add, mult)
        STT(s2T[:, :], modT[:, 24:32], 1.0, pp[:, PP["n2g"]:PP["n2g"] + 8], add, mult)
        for j in range(4):
            TS("dve", cbm[:, j, :], bm[:, :], ppc("cb", j), mult)
        ACT(cA[:, :], pp[:, PP["lam"]:PP["lam"] + 4], AF.Exp, scale=-1.0)
        ACT(cA[:, :], cA[:, :], AF.Ln, bias=1.0)
        TS("dve", cA[:, :], cA[:, :], -8.0, mult)
        TS("dve", omka[:, :], pp[:, PP["ka"]:PP["ka"] + 4], -1.0, mult, 1.0, add)
        P.barrier()
        es_s.close()

        xt = [sb(es_m, "xt%d" % i, [128, D], F32) for i in range(2)]
        ss = sb(es_m, "ss", [128, 2], F32)
        rs = sb(es_m, "rs", [128, 2], F32)
        xn = [sb(es_m, "xn%d" % i, [128, D], BF16) for i in range(2)]
        hT = [sb(es_m, "hT%d" % i, [128, 8, 128], BF16) for i in range(2)]
        pU = [sb(es_m, "pU%d" % i, [128, 128 + HALO], F32) for i in range(4)]
        pG = [sb(es_m, "pG%d" % i, [128, 128], F32) for i in range(4)]
        pR = [sb(es_m, "pR%d" % i, [128, 128 + HALO], F32) for i in range(14)]
        us = [sb(es_m, "us%d" % i, [128, 128], F32) for i in range(14)]
        hprev = sb(es_m, "hprev", [128, 4], F32)
        WC = sb(es_m, "WC", [128, 4], F32)
        nbt = sb(es_m, "nbt", [128, 4], F32)
        ARz = [sb(es_m, "ARz%d" % i, [128, 4, 2, 128], BF16) for i in range(2)]
        BK = sb(es_m, "BK", [128, 4, 2, 128], BF16)
        BKp = sb(es_m, "BKp", [128, 4, 2, 128], BF16)
        vTb = sb(es_m, "vTb", [128, 4, 128], BF16)
        Vt = sb(es_m, "Vt", [128, 512], BF16)
        Bt = sb(es_m, "Bt", [128, 512], BF16)
        Kt = sb(es_m, "Kt", [128, 512], BF16)
        bon = sb(es_m, "bon", [128, 4, 128], F32)
        gT = sb(es_m, "gT", [128, 4, 128], F32)
        M1 = [sb(es_m, "M1_%d" % g, [128, 4, 512], BF16) for g in range(2)]
        NTb = [[sb(es_m, "NTb%d_%d" % (g, i), [128, 512], BF16) for i in range(2)] for g in range(2)]
        Nb = [[sb(es_m, "Nb%d_%d" % (g, i), [128, 512], BF16) for i in range(2)] for g in range(2)]
        Zb = [[sb(es_m, "Zb%d_%d" % (g, i), [128, 512], BF16) for i in range(2)] for g in range(2)]
        Xb = sb(es_m, "Xb", [128, 2, 256], BF16)
        Ub = sb(es_m, "Ub", [128, 2, 256], BF16)
        STf = sb(es_m, "STf", [128, 4, 64], F32)
        STb = [sb(es_m, "STb%d" % i, [128, 4, 64], BF16) for i in range(2)]
        OT = sb(es_m, "OT", [128, 512], F32)
        yT = [sb(es_m, "yT%d" % i, [128, 8, 128], BF16) for i in range(2)]
        xr = [sb(es_m, "xr0", [128, D], F32)] * 2
        xo = [sb(es_m, "xo0", [128, D], F32)] * 2
        tmps = {}

        def tmpj(slot, j):
            if (slot, j) not in tmps:
                tmps[(slot, j)] = sb(es_m, "t_%d_%d" % (slot, j), [128, 128], F32)
            return tmps[(slot, j)]

        def lockstep(gens):
            gens = list(gens)
            while gens:
                alive = []
                for g_ in gens:
                    try:
                        next(g_)
                        alive.append(g_)
                    except StopIteration:
                        pass
                gens = alive

        for t_ in pU + pR:
            MSET("pool", t_[:, :], 0.0)
        for t_ in ARz:
            MSET("pool", t_[:, :, :, :], 0.0)
        MSET("dve", hprev[:, :], 0.0)
        MSET("dve", STf[:, :, :], 0.0)
        MSET("dve", STb[0][:, :, :], 0.0)

        chunks = [(pU[i], HALO) for i in range(4)] + [(pG[i], 0) for i in range(4)] + [(pR[i], HALO) for i in range(14)]
        stop = self.stop
        for blk in range(NBLK if stop > 0 else 0):
            par = blk % 2
            r0 = blk * 128
            bmc = bm[:, blk:blk + 1]
            DMA("sp", "x", xt[par][:, :], xs[r0:r0 + 128, :])
            ACT(xn[par][:, :], xt[par][:, :], AF.Square, accum=ss[:, par:par + 1])
            ACT(rs[:, par:par + 1], ss[:, par:par + 1], AF.Sqrt, scale=1.0 / D, bias=RMS_EPS)
            RECIP(rs[:, par:par + 1], rs[:, par:par + 1])
            TS("dve", xn[par][:, :], xt[par][:, :], rs[:, par:par + 1], mult)
            pb = psb()
            for kc in range(8):
                TR(pb[:, kc * 128:(kc + 1) * 128], xn[par][:, kc * 128:(kc + 1) * 128], identb[:, :])
            for kc in range(8):
                ACT(hT[par][:, kc, :], pb[:, kc * 128:(kc + 1) * 128], AF.Identity,
                    scale=s1T[:, kc:kc + 1], bias=modT[:, kc:kc + 1])
            if stop <= 1:
                continue
            for ch, (tile, hal) in enumerate(chunks):
                if hal:
                    CP("pool", tile[:, 0:HALO], tile[:, 128:128 + HALO])
                ps = psf()
                for kc in range(8):
                    MM(ps[:, 0:128], w_in_sb[:, kc, ch * 128:(ch + 1) * 128], hT[par][:, kc, :],
                       start=(kc == 0), stop=(kc == 7))
                if ch % 2 == 0:
                    TS("dve", tile[:, hal:hal + 128], ps[:, 0:128], bmc, mult)
                else:
                    ACT(tile[:, hal:hal + 128], ps[:, 0:128], AF.Identity, scale=bmc)
            yTp = yT[par]
            if stop <= 2:
                continue
            def lru_chain(j):
                ux = pU[j]
                xc, rg, ig, aa, a2, bx, hs, gl, yy = (tmpj(s_, j) for s_ in range(9))
                y2, rn = rg, ig
                TS("dve", xc[:, :], ux[:, 1:129], ppc("cw", j), mult, cbm[:, j, blk:blk + 1], add)
                yield
                for tap in range(1, 4):
                    STT(xc[:, :], ux[:, 1 + tap:129 + tap], ppc("cw", tap * 4 + j), xc[:, :], mult, add)
                    yield
                psA = psf()
                MM(psA[:, 0:128], bd[:, j, :], xc[:, :])
                MM(psA[:, 128:256], bd[:, 4 + j, :], xc[:, :])
                yield
                ACT(rg[:, :], psA[:, 0:128], AF.Sigmoid, bias=ppc("ba", j))
                ACT(ig[:, :], psA[:, 128:256], AF.Sigmoid, bias=ppc("bi", j))
                yield
                ACT(aa[:, :], rg[:, :], AF.Exp, scale=cA[:, j:j + 1])
                TT("pool", bx[:, :], ig[:, :], xc[:, :], mult)
                yield
                TT("pool", a2[:, :], aa[:, :], aa[:, :], mult)
                yield
                ACT(a2[:, :], a2[:, :], AF.Sqrt, scale=-1.0, bias=1.0)
                yield
                TT("pool", bx[:, :], bx[:, :], a2[:, :], mult)
                yield
                SCAN(hs[:, :], aa[:, :], bx[:, :], hprev[:, j:j + 1], mult, add)
                ACT(gl[:, :], pG[j][:, :], AF.Gelu_apprx_tanh)
                yield
                CP("pool", hprev[:, j:j + 1], hs[:, 127:128])
                TT("pool", yy[:, :], hs[:, :], gl[:, :], mult)
                yield
                TT("pool", y2[:, :], yy[:, :], yy[:, :], mult)
                yield
                psN = psf()
                MM(psN[:, 0:128], bones, y2[:, :])
                yield
                ACT(rn[:, :], psN[:, 0:128], AF.Sqrt, bias=RMS_EPS)
                yield
                RECIP(rn[:, :], rn[:, :])
                yield
                STT(yTp[:, j, :], yy[:, :], ppc("lg", j), rn[:, :], mult, mult)
                yield

            lockstep([lru_chain(j) for j in range(4)])
            if stop <= 3:
                continue
            for m in range(14):
                src = pR[m]
                dd = tmpj(13, m % 4)
                TT("pool", dd[:, :], src[:, HALO - 1:HALO + 127], src[:, HALO:HALO + 128], sub)
                STT(us[m][:, :], dd[:, :], ppc("mu", m), src[:, HALO:HALO + 128], mult, add)
            ACT(us[12][0:64, :], us[12][0:64, :], AF.Tanh)
            ACT(us[13][:, :], us[13][:, :], AF.Sigmoid)

            def rwkv_chain(j):
                (sgw, cs, eL, eLn, eLm, eLc, av, kk0, sqk, kkn, tq, k2, bt) = (tmpj(s_, j) for s_ in range(13))
                csm, nrm, rk = sgw, sqk, kk0
                psL = psf()
                MM(psL[:, 0:128], lup[:, 0, j * 128:(j + 1) * 128], us[12][:, :])
                MM(psL[:, 128:256], lup[:, 1, j * 128:(j + 1) * 128], us[12][:, :])
                MM(psL[:, 256:384], lup[:, 2, j * 128:(j + 1) * 128], us[13][:, :])
                yield
                ACT(sgw[:, :], psL[:, 0:128], AF.Sigmoid, bias=ppc("w0", j))
                ACT(av[:, :], psL[:, 128:256], AF.Sigmoid, bias=ppc("a0", j))
                ACT(gT[:, j, :], psL[:, 256:384], AF.Identity)
                TS("dve", kk0[:, :], us[4 + j][:, :], ppc("kk", j), mult)
                yield
                SCAN(cs[:, :], ones, sgw[:, :], 0.0, mult, add)
                TT("pool", sqk[:, :], kk0[:, :], kk0[:, :], mult)
                yield
                psK = psf()
                MM(psK[:, 0:128], bones, sqk[:, :])
                ACT(eL[:, :], cs[:, :], AF.Exp, scale=-LDK)
                TS("dve", tq[:, :], av[:, :], ppc("ka", j), mult, omka[:, j:j + 1], add)
                yield
                ACT(eLn[:, :], cs[:, :], AF.Exp, scale=LDK)
                TT("pool", csm[:, :], cs[:, :], sgw[:, :], sub)
                TS("dve", nbt[:, j:j + 1], cs[:, 127:128], -LDK, mult)
                yield
                ACT(eLm[:, :], csm[:, :], AF.Exp, scale=-LDK)
                CP("pool", WC[:, j:j + 1], eL[:, 127:128])
                TT("pool", k2[:, :], us[4 + j][:, :], tq[:, :], mult)
                yield
                ACT(eLc[:, :], cs[:, :], AF.Exp, scale=LDK, bias=nbt[:, j:j + 1])
                yield
                ACT(nrm[:, :], psK[:, 0:128], AF.Sqrt, scale=64.0)
                yield
                TS("dve", nrm[:, :], nrm[:, :], 1e-12, ALU.max)
                yield
                RECIP(nrm[:, :], nrm[:, :])
                yield
                TT("pool", kkn[:, :], kk0[:, :], nrm[:, :], mult)
                CP("dve", vTb[:, j, :], us[8 + j][:, :])
                yield
                TT("pool", bt[:, :], kkn[:, :], av[:, :], mult)
                for hp in range(2):
                    hs_ = slice(hp * 64, hp * 64 + 64)
                    STT(ARz[hp][hs_, j, 0, :], kkn[hs_, :], -1.0, eLm[hs_, :], mult, mult)
                yield
                for hp in range(2):
                    hs_ = slice(hp * 64, hp * 64 + 64)
                    TT("pool", ARz[hp][hs_, j, 1, :], us[j][hs_, :], eL[hs_, :], mult)
                STT(rk[:, :], us[j][:, :], ppc("rk", j), k2[:, :], mult, mult)
                yield
                TT("pool", BK[:, j, 0, :], bt[:, :], eLn[:, :], mult)
                psBn = psf()
                MM(psBn[:, 0:128], bones, rk[:, :])
                yield
                TT("pool", BK[:, j, 1, :], k2[:, :], eLn[:, :], mult)
                STT(bon[:, j, :], psBn[:, 0:128], 64.0, us[8 + j][:, :], mult, mult)
                yield
                TT("pool", BKp[:, j, 0, :], bt[:, :], eLc[:, :], mult)
                yield
                TT("pool", BKp[:, j, 1, :], k2[:, :], eLc[:, :], mult)
                yield

            lockstep([rwkv_chain(j) for j in range(4)])
            if stop <= 4:
                continue
            for which, dst in ((None, Vt), (0, Bt), (1, Kt)):
                pb = psb()
                for j in range(4):
                    srcv = vTb[:, j, :] if which is None else BKp[:, j, which, :]
                    TR(pb[:, j * 128:(j + 1) * 128], srcv, identb[:, :])
                CP("act" if which is None else "dve", dst[:, :], pb[:, 0:512])
            cur, nxt = STb[blk % 2], STb[(blk + 1) % 2]

            def hd(g, i):
                h = 4 * g + i
                return h, h // 2, slice((h % 2) * 64, (h % 2) * 64 + 64)

            for g in range(2):
                for i in range(4):
                    h, j, psl = hd(g, i)
                    hp = h % 2
                    psAb = psf()
                    MM(psAb[:, 0:256], BK[:, j, 0, :], ARz[hp][:, j, :, :])
                    TT("dve", M1[g][:, i, 0:256], psAb[:, 0:256], maskSI, mult)
                    psAk = psf()
                    MM(psAk[:, 0:256], BK[:, j, 1, :], ARz[hp][:, j, :, :])
                    TT("dve", M1[g][:, i, 256:512], psAk[:, 0:256], maskSI, mult)
                pbt = psb()
                for i in range(4):
                    TR(pbt[:, i * 128:(i + 1) * 128], M1[g][:, i, 0:128], identb[:, :])
                CP("act", NTb[g][0][:, :], pbt[:, 0:512])
                for i in range(4):
                    TT("pool", Zb[g][0][:, i * 128:(i + 1) * 128], M1[g][:, i, 0:128], identb[:, :], add)
            if stop <= 5:
                continue
            Ncur = [[M1[g][:, i, 0:128] for i in range(4)] for g in range(2)]
            NTcur = [[NTb[g][0][:, i * 128:(i + 1) * 128] for i in range(4)] for g in range(2)]
            Zcur = [Zb[g][0] for g in range(2)]
            for lv in range(1, 7):
                pq = lv % 2
                psNTn = [psf() for g in range(2)]
                for g in range(2):
                    for i in range(4):
                        MM(psNTn[g][:, i * 128:(i + 1) * 128], Ncur[g][i], NTcur[g][i])
                if lv < 6:
                    psNn = [psf() for g in range(2)]
                    for g in range(2):
                        for i in range(4):
                            MM(psNn[g][:, i * 128:(i + 1) * 128], NTcur[g][i], Ncur[g][i])
                for g in range(2):
                    CP("act", NTb[g][pq][:, :], psNTn[g][:, 0:512])
                if lv < 6:
                    for g in range(2):
                        CP("dve", Nb[g][pq][:, :], psNn[g][:, 0:512])
                NTn = [[NTb[g][pq][:, i * 128:(i + 1) * 128] for i in range(4)] for g in range(2)]
                psZ = [psf() for g in range(2)]
                for g in range(2):
                    for i in range(4):
                        zc = Zcur[g][:, i * 128:(i + 1) * 128]
                        MM(psZ[g][:, i * 128:(i + 1) * 128], identb[:, :], zc, start=True, stop=False)
                        MM(psZ[g][:, i * 128:(i + 1) * 128], NTn[g][i], zc, start=False, stop=True)
                for g in range(2):
                    CP("act" if g == 0 else "dve", Zb[g][pq][:, :], psZ[g][:, 0:512])
                Zcur = [Zb[g][pq] for g in range(2)]
                NTcur = NTn
                if lv < 6:
                    Ncur = [[Nb[g][pq][:, i * 128:(i + 1) * 128] for i in range(4)] for g in range(2)]
            if stop <= 6:
                continue
            psX = [psf() for g in range(2)]
            for g in range(2):
                for i in range(4):
                    h, j, psl = hd(g, i)
                    MM(psX[g][:, i * 64:(i + 1) * 64], M1[g][:, i, 256:384], Vt[:, h * 64:(h + 1) * 64], start=True, stop=False)
                    MM(psX[g][:, i * 64:(i + 1) * 64], ARz[h % 2][:, j, 0, :], cur[:, j, :], start=False, stop=True)
            for g in range(2):
                CP("act" if g == 0 else "dve", Xb[:, g, :], psX[g][:, 0:256])
            psU = [psf() for g in range(2)]
            for g in range(2):
                for i in range(4):
                    MM(psU[g][:, i * 64:(i + 1) * 64], Zcur[g][:, i * 128:(i + 1) * 128], Xb[:, g, i * 64:(i + 1) * 64])
            for g in range(2):
                CP("act" if g == 0 else "dve", Ub[:, g, :], psU[g][:, 0:256])
            psO = [psf() for g in range(2)]
            psS = psf()
            for g in range(2):
                for i in range(4):
                    h, j, psl = hd(g, i)
                    ip = (i // 2) * 2
                    ubp = Ub[:, g, ip * 64:ip * 64 + 128]
                    vvp = Vt[:, j * 128:(j + 1) * 128]
                    ub = Ub[:, g, i * 64:(i + 1) * 64]
                    vv = Vt[:, h * 64:(h + 1) * 64]
                    oc = slice(i * 128, (i + 1) * 128)
                    MM(psO[g][:, oc], ubp, M1[g][:, i, 128:256], start=True, stop=False)
                    MM(psO[g][:, oc], vvp, M1[g][:, i, 384:512], start=False, stop=False)
                    MM(psO[g][psl, oc], cur[psl, j, :], ARz[h % 2][psl, j, 1, :], start=False, stop=True)
                    MM(psS[:, h * 64:(h + 1) * 64], Bt[:, j * 128:(j + 1) * 128], ub, start=True, stop=False)
                    MM(psS[:, h * 64:(h + 1) * 64], Kt[:, j * 128:(j + 1) * 128], vv, start=False, stop=True)
            for h in range(8):
                j, psl = h // 2, slice((h % 2) * 64, (h % 2) * 64 + 64)
                STT(STf[psl, j, :], STf[psl, j, :], WC[psl, j:j + 1], psS[psl, h * 64:(h + 1) * 64], mult, add)
            CP("pool", nxt[:, :, :], STf[:, :, :])
            for g in range(2):
                for i in range(4):
                    h, j, psl = hd(g, i)
                    CP("act" if i % 2 == 0 else "dve", OT[psl, j * 128:(j + 1) * 128], psO[g][psl, i * 128:(i + 1) * 128])
            if stop <= 7:
                continue
            def post_chain(j):
                cen, sq2, sd, on = (tmpj(s_, j) for s_ in range(4))
                psM = psf()
                MM(psM[:, 0:128], bones, OT[:, j * 128:(j + 1) * 128])
                yield
                TT("dve", cen[:, :], OT[:, j * 128:(j + 1) * 128], psM[:, 0:128], sub)
                yield
                TT("pool", sq2[:, :], cen[:, :], cen[:, :], mult)
                yield
                psV = psf()
                MM(psV[:, 0:128], bones, sq2[:, :])
                yield
                ACT(sd[:, :], psV[:, 0:128], AF.Sqrt, bias=GN_EPS)
                yield
                RECIP(sd[:, :], sd[:, :])
                yield
                TT("pool", on[:, :], cen[:, :], sd[:, :], mult)
                yield
                TS("dve", on[:, :], on[:, :], ppc("lnw", j), mult, ppc("lnb", j), add)
                yield
                TT("pool", on[:, :], on[:, :], bon[:, j, :], add)
                yield
                TT("pool", yTp[:, 4 + j, :], on[:, :], gT[:, j, :], mult)
                yield

            lockstep([post_chain(j) for j in range(4)])
            if stop <= 8:
                continue
            if blk >= NBLK // 2:
                tok0 = (blk - NBLK // 2) * 128
                DMA("sp", "x", xr[par][:, :], xs[r0:r0 + 128, :])
                for nh in range(2):
                    ps = psf()
                    cs_ = slice(nh * 512, (nh + 1) * 512)
                    for kc in range(8):
                        MM(ps[:, 0:512], yTp[:, kc, :], w_out_sb[:, kc, cs_], start=(kc == 0), stop=(kc == 7))
                    TT("dve", xo[par][:, cs_], ps[:, 0:512], g1b[:, cs_], mult)
                    TT("pool", xo[par][:, cs_], xo[par][:, cs_], xr[par][:, cs_], add)
                DMA("sp", "xn", xnew[tok0:tok0 + 128, :], xo[par][:, :])
        P.barrier()
        es_m.close()
        self.moe(es, es_all, xnew, outd, w1d, w3d, w2d, s2T, modT, g2b, bcB, wr, cst, identb)
        fin = [(k, P.count[k]) for k in P.semkeys]
        P.wait_all("sp", fin)
        P.emit()
        es.close()
        es_all.close()
        return nc

    def moe(self, es, es_all, xnew, outd, w1d, w3d, w2d, s2T, modT, g2b, bcB, wr, cst, identb):
        nc, P, NOWN = self.nc, self.P, self.NOWN
        ACT, TS, STT, TT, CP, MM, TR, DMA, RECIP, MSET = (self.ACT, self.TS, self.STT, self.TT, self.CP, self.MM,
                                                          self.TR, self.DMA, self.RECIP, self.MSET)
        mult, add, sub = ALU.mult, ALU.add, ALU.subtract
        sb, psf = self.sb, self.psf
        es_e = ExitStack()
        NTT = NOWN // 128
        fgb = bcB[:, 0:D]
        xl = [sb(es_e, "xl%d" % i, [128, D], F32) for i in range(2)]
        ot = [sb(es_e, "ot%d" % i, [128, D], F32) for i in range(2)]
        sqj = sb(es_e, "sqj2", [128, D], BF16)
        ss = sb(es_e, "ss2", [128, 2], F32)
        rs = sb(es_e, "rs2", [128, 2], F32)
        if self.debug == "nomoe":
            for tt in range(NTT):
                par = tt % 2
                DMA("sp", "x", xl[par][:, :], xnew[tt * 128:(tt + 1) * 128, :])
                ACT(sqj[:, :], xl[par][:, :], AF.Square, accum=ss[:, par:par + 1])
                ACT(rs[:, par:par + 1], ss[:, par:par + 1], AF.Sqrt, scale=1.0 / D, bias=RMS_EPS)
                RECIP(rs[:, par:par + 1], rs[:, par:par + 1])
                STT(ot[par][:, :], xl[par][:, :], rs[:, par:par + 1], fgb, mult, mult)
                DMA("sp", "o", outd[tt * 128:(tt + 1) * 128, :], ot[par][:, :])
            es_e.close()
            return
        es_e.close()
        RED, MAX8 = self.RED, self.MAX8
        ident = cst[:, C_ID:C_ID + 128]
        iota = cst[:, C_IOTA:C_IOTA + CAP]
        brow = bcB[:, D:D + 36]
        NH = 2 if NOWN >= 2048 else 1
        HT = NOWN // NH
        NT_ = HT // 128
        NSH = CAP // 128
        es_h = ExitStack()
        h2tm = sb(es_h, "h2tm", [128, NT_, D], BF16)
        acc = [sb(es_h, "acc%d" % i, [128, D], F32) for i in range(NT_)]
        gate = sb(es_h, "gate", [128, NT_, NE], F32)
        mskb = sb(es_h, "mskb", [128, NT_, NE], BF16)
        mskf = sb(es_h, "mskf", [128, NT_, NE], F32)
        posm = sb(es_h, "posm", [128, NT_, NE], F32)
        onesb = sb(es_h, "onesb", [128, 128], BF16)
        triSb = sb(es_h, "triSb", [128, 128], BF16)
        CP("dve", onesb[:, :], cst[:, C_ONES:C_ONES + 128])
        CP("dve", triSb[:, :], cst[:, C_TRIS:C_TRIS + 128])
        for hf in range(NH):
            t0 = hf * HT
            es1 = ExitStack()
            xl = [sb(es1, "m1xl%d" % i, [128, D], F32) for i in range(2)]
            xn2 = [sb(es1, "m1xn%d" % i, [128, D], F32) for i in range(2)]
            h2f = sb(es1, "h2f", [128, 8, 128], F32)
            h2Tt = sb(es1, "h2Tt", [128, 8, 128], BF16)
            sqj = sb(es1, "m1sq", [128, D], BF16)
            sm = {n: sb(es1, "m1_" + n, [128, w], F32) for n, w in (
                ("ss", 2), ("rs", 2), ("lg", 36), ("gmax", 1), ("ngmax", 1), ("e4", 4), ("gsum", 1), ("oh4", 4),
                ("ig8", 8), ("m8", 8), ("sel", 8), ("nv1", 1), ("e8", 8), ("w8", 8), ("den", 1), ("coef", 1))}
            for tt in range(NT_):
                par = tt % 2
                r0 = t0 + tt * 128
                DMA("sp", "x", xl[par][:, :], xnew[r0:r0 + 128, :])
                ACT(sqj[:, :], xl[par][:, :], AF.Square, accum=sm["ss"][:, par:par + 1])
                ACT(sm["rs"][:, par:par + 1], sm["ss"][:, par:par + 1], AF.Sqrt, scale=1.0 / D, bias=RMS_EPS)
                RECIP(sm["rs"][:, par:par + 1], sm["rs"][:, par:par + 1])
                TS("dve", xn2[par][:, :], xl[par][:, :], sm["rs"][:, par:par + 1], mult)
                pst = [psf(), psf()]
                for kc in range(8):
                    TR(pst[kc // 4][:, (kc % 4) * 128:(kc % 4 + 1) * 128], xn2[par][:, kc * 128:(kc + 1) * 128], ident)
                for kc in range(8):
                    pv = pst[kc // 4][:, (kc % 4) * 128:(kc % 4 + 1) * 128]
                    ACT(h2Tt[:, kc, :], pv, AF.Identity, scale=s2T[:, kc:kc + 1], bias=modT[:, 16 + kc:17 + kc])
                    TS("dve", h2f[:, kc, :], pv, s2T[:, kc:kc + 1], mult, modT[:, 16 + kc:17 + kc], add)
                pbt = self.psb()
                for kc in range(8):
                    TR(pbt[:, kc * 128:(kc + 1) * 128], h2Tt[:, kc, :], identb[:, :])
                CP("act", h2tm[:, tt, :], pbt[:, 0:D])
                psR = psf()
                for kc in range(8):
                    MM(psR[:, 0:36], h2f[:, kc, :], wr[:, kc, :], start=(kc == 0), stop=(kc == 7))
                lg = sm["lg"]
                TT("dve", lg[:, :], psR[:, 0:36], brow, add)
                RED(sm["gmax"][:, :], lg[:, 0:4], ALU.max)
                TS("dve", sm["ngmax"][:, :], sm["gmax"][:, :], -1.0, mult)
                ACT(sm["e4"][:, :], lg[:, 0:4], AF.Exp, bias=sm["ngmax"][:, 0:1], accum=sm["gsum"][:, 0:1])
                TS("dve", sm["oh4"][:, :], lg[:, 0:4], sm["gmax"][:, 0:1], ALU.is_ge)
                TS("dve", sm["ig8"][:, :], lg[:, 4:12], sm["oh4"][:, 0:1], mult)
                for g in range(1, 4):
                    STT(sm["ig8"][:, :], lg[:, 4 + 8 * g:12 + 8 * g], sm["oh4"][:, g:g + 1], sm["ig8"][:, :], mult, add)
                MAX8(sm["m8"][:, :], sm["ig8"][:, :])
                TS("dve", sm["sel"][:, :], sm["ig8"][:, :], sm["m8"][:, 1:2], ALU.is_ge)
                TS("dve", sm["nv1"][:, :], sm["m8"][:, 0:1], -1.0, mult)
                ACT(sm["e8"][:, :], sm["ig8"][:, :], AF.Exp, bias=sm["nv1"][:, 0:1])
                TT("dve", sm["w8"][:, :], sm["e8"][:, :], sm["sel"][:, :], mult)
                RED(sm["den"][:, :], sm["w8"][:, :], ALU.add)
                TT("dve", sm["coef"][:, :], sm["den"][:, :], sm["gsum"][:, :], mult)
                RECIP(sm["coef"][:, :], sm["coef"][:, :])
                TS("dve", sm["w8"][:, :], sm["w8"][:, :], sm["coef"][:, 0:1], mult)
                for g in range(4):
                    TS("dve", gate[:, tt, 8 * g:8 * g + 8], sm["w8"][:, :], sm["oh4"][:, g:g + 1], mult)
                    TS("dve", mskf[:, tt, 8 * g:8 * g + 8], sm["sel"][:, :], sm["oh4"][:, g:g + 1], mult)
                CP("dve", mskb[:, tt, :], mskf[:, tt, :])
                MSET("pool", acc[tt][:, :], 0.0)
            for tt in range(NT_):
                psP = psf()
                for u in range(tt):
                    MM(psP[:, 0:NE], onesb[:, :], mskb[:, u, :], start=(u == 0), stop=False)
                MM(psP[:, 0:NE], triSb[:, :], mskb[:, tt, :], start=(tt == 0), stop=True)
                STT(posm[:, tt, :], psP[:, 0:NE], 1.0, mskf[:, tt, :], add, mult)
                TS("dve", posm[:, tt, :], posm[:, tt, :], -1.0, add)
            P.barrier()
            es1.close()
            es2 = ExitStack()
            w1b = [sb(es2, "w1b%d" % i, [128, 8, DE], BF16) for i in range(2)]
            w3b = [sb(es2, "w3b%d" % i, [128, 8, DE], BF16) for i in range(2)]
            w2b = [sb(es2, "w2b%d" % i, [128, 4, D], BF16) for i in range(2)]
            Sel = sb(es2, "Sel", [128, NT_, CAP], BF16)
            SelT = sb(es2, "SelT", [128, NT_ * NSH * 128], BF16)
            xsT = sb(es2, "xsT", [128, 8, CAP], BF16)
            hidT = sb(es2, "hidT", [128, 4, CAP], BF16)
            ye = sb(es2, "ye", [128, NSH, D], BF16)
            s1t = [sb(es2, "s1t%d" % i, [128, CAP], F32) for i in range(2)]
            for e in range(self.ne if self.mstop > 1 else 0):
                wb = e % 2
                DMA("pool", "wm", w1b[wb][:, :, :], V(w1d, w1d.t[e].rearrange("(k p) n -> p k n", p=128)))
                DMA("pool", "wm", w3b[wb][:, :, :], V(w3d, w3d.t[e].rearrange("(k p) n -> p k n", p=128)))
                DMA("pool", "wm", w2b[wb][:, :, :], V(w2d, w2d.t[e].rearrange("(k p) n -> p k n", p=128)))
                if self.mstop <= 2:
                    continue
                for tt in range(NT_):
                    TS("dve", Sel[:, tt, :], iota, posm[:, tt, e:e + 1], ALU.is_equal)
                for kc in range(8):
                    psG = psf()
                    for tt in range(NT_):
                        MM(psG[:, 0:CAP], h2tm[:, tt, kc * 128:(kc + 1) * 128], Sel[:, tt, :],
                           start=(tt == 0), stop=(tt == NT_ - 1))
                    CP("act" if kc % 2 == 0 else "dve", xsT[:, kc, :], psG[:, 0:CAP])
                nblk_ = NT_ * NSH
                for q in range(0, nblk_, 8):
                    pbt = self.psb()
                    nb_ = min(8, nblk_ - q)
                    for r in range(nb_):
                        tt, sh = (q + r) // NSH, (q + r) % NSH
                        TR(pbt[:, r * 128:(r + 1) * 128], Sel[:, tt, sh * 128:(sh + 1) * 128], identb[:, :])
                    CP("act", SelT[:, q * 128:(q + nb_) * 128], pbt[:, 0:nb_ * 128])
                if self.mstop <= 3:
                    continue
                for hc in range(4):
                    ps1 = psf()
                    for kc in range(8):
                        MM(ps1[:, 0:CAP], w1b[wb][:, kc, hc * 128:(hc + 1) * 128], xsT[:, kc, :],
                           start=(kc == 0), stop=(kc == 7))
                    ps3 = psf()
                    for kc in range(8):
                        MM(ps3[:, 0:CAP], w3b[wb][:, kc, hc * 128:(hc + 1) * 128], xsT[:, kc, :],
                           start=(kc == 0), stop=(kc == 7))
                    st_ = s1t[hc % 2]
                    ACT(st_[:, :], ps1[:, 0:CAP], AF.Silu)
                    TT("dve", hidT[:, hc, :], st_[:, :], ps3[:, 0:CAP], mult)
                for sh in range(NSH):
                    for nh in range(2):
                        cs_ = slice(nh * 512, (nh + 1) * 512)
                        psY = psf()
                        for hc in range(4):
                            MM(psY[:, 0:512], hidT[:, hc, sh * 128:(sh + 1) * 128], w2b[wb][:, hc, cs_],
                               start=(hc == 0), stop=(hc == 3))
                        CP("act", ye[:, sh, cs_], psY[:, 0:512])
                if self.mstop <= 4:
                    continue
                for tt in range(NT_):
                    for nh in range(2):
                        cs_ = slice(nh * 512, (nh + 1) * 512)
                        psS = psf()
                        for sh in range(NSH):
                            o_ = (tt * NSH + sh) * 128
                            MM(psS[:, 0:512], SelT[:, o_:o_ + 128], ye[:, sh, cs_], start=(sh == 0), stop=(sh == NSH - 1))
                        STT(acc[tt][:, cs_], psS[:, 0:512], gate[:, tt, e:e + 1], acc[tt][:, cs_], mult, add)
            P.barrier()
            es2.close()
            es3 = ExitStack()
            xl = [sb(es3, "m3xl%d" % i, [128, D], F32) for i in range(2)]
            ot = [sb(es3, "m3ot%d" % i, [128, D], F32) for i in range(2)]
            sqj = sb(es3, "m3sq", [128, D], BF16)
            ss = sb(es3, "m3ss", [128, 2], F32)
            rs = sb(es3, "m3rs", [128, 2], F32)
            for tt in range(NT_):
                par = tt % 2
                r0 = t0 + tt * 128
                DMA("sp", "x", xl[par][:, :], xnew[r0:r0 + 128, :])
                TT("dve", acc[tt][:, :], acc[tt][:, :], g2b[:, :], mult)
                TT("pool", xl[par][:, :], xl[par][:, :], acc[tt][:, :], add)
                ACT(sqj[:, :], xl[par][:, :], AF.Square, accum=ss[:, par:par + 1])
                ACT(rs[:, par:par + 1], ss[:, par:par + 1], AF.Sqrt, scale=1.0 / D, bias=RMS_EPS)
                RECIP(rs[:, par:par + 1], rs[:, par:par + 1])
                STT(ot[par][:, :], xl[par][:, :], rs[:, par:par + 1], fgb, mult, mult)
                DMA("sp", "o", outd[r0:r0 + 128, :], ot[par][:, :])
            P.barrier()
            es3.close()
        es_h.close()


def _cols(v, n):
    return np.ascontiguousarray(np.asarray(v, np.float32).reshape(n, 128).T)


def _host_consts():
    c = np.zeros((128, NCONST), np.float32)
    c[:, C_ID:C_ID + 128] = np.eye(128, dtype=np.float32)
    bo = np.zeros((128, 128), np.float32)
    bo[:64, :64] = 1.0 / 64
    bo[64:, 64:] = 1.0 / 64
    c[:, C_BONES:C_BONES + 128] = bo
    s = np.arange(128)[:, None]
    t = np.arange(128)[None, :]
    c[:, C_TRIS:C_TRIS + 128] = (s < t)
    c[:, C_TRII:C_TRII + 128] = (s <= t)
    c[:, C_ONES:C_ONES + 128] = 1.0
    for i in range(4):
        c[:, C_TRIL4 + i * 128:C_TRIL4 + (i + 1) * 128] = (s > t)
    c[:, C_IOTA:C_IOTA + CAP] = np.arange(CAP, dtype=np.float32)[None, :]
    return c


def _prep_inputs(inputs, T, with_moe=True, ne=NE):
    f = lambda k: np.asarray(inputs[k], np.float32)
    x = f("x")
    B = x.shape[0]
    half = T // 2
    pp = np.zeros((128, NPP), np.float32)

    def put(name, v, n):
        pp[:, PP[name]:PP[name] + n] = _cols(v, n)

    b_ada = f("b_ada")[0]
    put("n1g", f("norm1_g")[0], 8)
    put("n2g", f("norm2_g")[0], 8)
    put("bsh1", b_ada[0:1024], 8)
    put("bsc1", b_ada[1024:2048], 8)
    put("bsh2", b_ada[3072:4096], 8)
    put("bsc2", b_ada[4096:5120], 8)
    cw = f("conv_w")[0]
    for tap in range(4):
        pp[:, PP["cw"] + tap * 4:PP["cw"] + tap * 4 + 4] = _cols(cw[tap], 4)
    put("cb", f("conv_b")[0], 4)
    put("ba", f("lru_ba")[0].reshape(-1), 4)
    put("bi", f("lru_bi")[0].reshape(-1), 4)
    put("lam", f("lru_lam")[0], 4)
    put("lg", f("lru_norm_g")[0], 4)
    put("mu", f("tok_mu")[0], 14)
    put("w0", f("w0")[0], 4)
    put("a0", f("a0")[0], 4)
    put("kk", f("k_k")[0], 4)
    put("ka", f("k_a")[0], 4)
    put("rk", f("r_k")[0].reshape(-1), 4)
    put("lnw", f("ln_x_w")[0], 4)
    put("lnb", f("ln_x_b")[0], 4)
    wa, wi = f("lru_wa")[0], f("lru_wi")[0]
    bd = np.zeros((128, 8, 128), np.float32)
    for j in range(4):
        for hp in range(2):
            sl = slice(hp * 64, hp * 64 + 64)
            bd[sl, j, sl] = wa[2 * j + hp]
            bd[sl, 4 + j, sl] = wi[2 * j + hp]
    lup = np.zeros((128, 3, 512), np.float32)
    lup[0:64, 0] = f("w_up")[0]
    lup[64:128, 1] = f("a_up")[0]
    lup[:, 2] = f("g_up")[0]
    bcA = np.ascontiguousarray(np.broadcast_to(np.concatenate([b_ada[2048:3072], b_ada[5120:6144]])[None, :], (128, 2048)))
    bcB = np.ascontiguousarray(np.broadcast_to(
        np.concatenate([f("final_g"), f("b_grp")[0], f("b_exp")[0]])[None, :], (128, 1024 + 36)))
    wrr = np.concatenate([f("w_grp")[0], f("w_exp")[0]], axis=1)
    wr = np.ascontiguousarray(wrr.reshape(8, 128, 36).transpose(1, 0, 2))
    consts = _host_consts()
    shared = {"w_ada": f("w_ada")[0], "pp": pp, "w_in": f("w_in")[0], "w_out": f("w_out")[0], "bd": bd, "lup": lup,
              "bcA": bcA, "bcB": bcB, "wr": wr, "consts": consts}
    if with_moe:
        shared.update({"w1": f("w1")[0][:ne], "w3": f("w3")[0][:ne], "w2": f("w2")[0][:ne]})
    c = f("c")
    in_maps = []
    nblk = T // 128
    for core in range(2 * B):
        b, hf = core // 2, core % 2
        m = dict(shared)
        bmk = np.ones((128, nblk), np.float32)
        if hf == 0:
            xs = np.concatenate([np.zeros((half, D), np.float32), x[b, :half]], axis=0)
            bmk[:, :nblk // 2] = 0.0
        else:
            xs = x[b]
        m["xs"] = np.ascontiguousarray(xs)
        m["cT"] = _cols(c[b], 8)
        m["blkmask"] = bmk
        in_maps.append(m)
    return in_maps


_NC_CACHE = {}


def kernel(_debug=None, _stop=99, _ne=NE, _mstop=99, **inputs):
    x = np.asarray(inputs["x"])
    B, T, _ = x.shape
    in_maps = _prep_inputs(inputs, T, with_moe=(_debug != "nomoe"), ne=_ne)
    key = (T, _debug, _stop, _ne, _mstop)
    if key not in _NC_CACHE:
        _NC_CACHE[key] = KB(T, debug=_debug, stop=_stop, ne=_ne, mstop=_mstop).build()
    nc = _NC_CACHE[key]
    res = run_bass_kernel_spmd(nc, in_maps, core_ids=list(range(2 * B)))
    out = np.zeros((B, T, D), np.float32)
    half = T // 2
    for core in range(2 * B):
        b, hf = core // 2, core % 2
        out[b, hf * half:(hf + 1) * half] = res.results[core]["out"]
    return out
```
